# Optimizing a Trainium2 kernel written in Bass

```python
import math
import jax, jax.numpy as jnp
from jax import lax
import numpy as np


D_MODEL = 1024
BATCH = 16
SEQ = 2048
DEPTH = 2

CHUNK = 64
HEAD_DIM = 64
ROPE_THETA = 10000.0
EPS = 1e-6
D_FF = 2816
Q_BLOCK = 128
DSA_Q_BLOCK = 32
A_HEADS = D_MODEL // (4 * HEAD_DIM)
A_VDIM = 2 * HEAD_DIM
B_HEADS = D_MODEL // (2 * HEAD_DIM)
IDX_HEADS = 8
IDX_DIM = 32
TOPK_MAX = 256
C_HEADS = D_MODEL // HEAD_DIM
C_LEFT_CHUNKS = 8
MAX_REL = 256
REL_TABLE = MAX_REL + CHUNK

MIX_WIDTH = A_HEADS * A_VDIM + B_HEADS * HEAD_DIM
EVEN_SIZES = (2 * A_HEADS * HEAD_DIM, 2 * A_HEADS * HEAD_DIM, A_HEADS * A_VDIM,
              B_HEADS * HEAD_DIM, B_HEADS * HEAD_DIM, B_HEADS * HEAD_DIM,
              IDX_HEADS * IDX_DIM, IDX_DIM, IDX_HEADS)
EVEN_PROJ = sum(EVEN_SIZES)
EVEN_SPLITS = [sum(EVEN_SIZES[:i + 1]) for i in range(len(EVEN_SIZES) - 1)]

kernel_name = "hybrid_chunk_causal_diff_dsa_band"


def rmsnorm(x, g):
    xf = x.astype(jnp.float32)
    y = xf * lax.rsqrt(jnp.mean(xf * xf, axis=-1, keepdims=True) + EPS)
    return (y * g.astype(jnp.float32)).astype(x.dtype)


def rope_tables(seq, dim):
    inv = ROPE_THETA ** (-jnp.arange(0, dim, 2, dtype=jnp.float32) / dim)
    ang = jnp.arange(seq, dtype=jnp.float32)[:, None] * inv[None, :]
    return jnp.cos(ang), jnp.sin(ang)


def apply_rope(x, cos, sin):
    xf = x.astype(jnp.float32)
    x1, x2 = jnp.split(xf, 2, axis=-1)
    c = cos[:, None, :]
    s = sin[:, None, :]
    return jnp.concatenate([x1 * c - x2 * s, x2 * c + x1 * s], axis=-1).astype(x.dtype)


def masked_softmax(s, mask):
    return jax.nn.softmax(jnp.where(mask, s, -jnp.inf), axis=-1)


def swiglu(h, wg, wu, wd):
    return (jax.nn.silu(h @ wg) * (h @ wu)) @ wd


def diff_attention(q, k, v, lam, subln, lambda_init):
    B_, S_ = q.shape[0], q.shape[1]
    nb = S_ // Q_BLOCK
    qb = q.reshape(B_, nb, Q_BLOCK, A_HEADS, 2, HEAD_DIM).swapaxes(0, 1)
    kf = k.astype(jnp.float32)
    vf = v.astype(jnp.float32)
    kchunk = jnp.arange(S_) // CHUNK
    scale = HEAD_DIM ** -0.5

    def one_block(args):
        bi, qblk = args
        qpos = bi * Q_BLOCK + jnp.arange(Q_BLOCK)
        mask = kchunk[None, :] <= (qpos // CHUNK)[:, None]
        s = jnp.einsum('bqhcd,bkhcd->bhcqk', qblk.astype(jnp.float32), kf) * scale
        p = masked_softmax(s, mask)
        w = p[:, :, 0] - lam * p[:, :, 1]
        return jnp.einsum('bhqk,bkhe->bqhe', w, vf)

    o = lax.map(one_block, (jnp.arange(nb), qb))
    o = o.swapaxes(0, 1).reshape(B_, S_, A_HEADS, A_VDIM)
    o = rmsnorm(o, subln) * (1.0 - lambda_init)
    return o.reshape(B_, S_, A_HEADS * A_VDIM).astype(v.dtype)


def dsa_attention(q, k, v, qi, ki, wi, topk):
    B_, S_ = q.shape[0], q.shape[1]
    nb = S_ // DSA_Q_BLOCK
    qb = q.reshape(B_, nb, DSA_Q_BLOCK, B_HEADS, HEAD_DIM).swapaxes(0, 1)
    qib = qi.reshape(B_, nb, DSA_Q_BLOCK, IDX_HEADS, IDX_DIM).swapaxes(0, 1)
    wib = wi.reshape(B_, nb, DSA_Q_BLOCK, IDX_HEADS).swapaxes(0, 1)
    kf = k.astype(jnp.float32)
    vf = v.astype(jnp.float32)
    kif = ki.astype(jnp.float32)
    kchunk = jnp.arange(S_) // CHUNK
    scale = HEAD_DIM ** -0.5
    gather = jax.vmap(lambda kk, ii: kk[ii])

    def one_block(args):
        bi, qblk, qiblk, wiblk = args
        qpos = bi * DSA_Q_BLOCK + jnp.arange(DSA_Q_BLOCK)
        qchunk = qpos // CHUNK
        rel = jax.nn.relu(jnp.einsum('bqhd,bkd->bqhk', qiblk.astype(jnp.float32), kif))
        score = jnp.einsum('bqhk,bqh->bqk', rel, wiblk.astype(jnp.float32))
        mask = kchunk[None, :] <= qchunk[:, None]
        score = jnp.where(mask[None], score, -jnp.inf)
        _, idx = lax.top_k(score, topk)
        valid = (idx // CHUNK) <= qchunk[None, :, None]
        kg = gather(kf, idx)
        vg = gather(vf, idx)
        s = jnp.einsum('bqhd,bqkhd->bhqk', qblk.astype(jnp.float32), kg) * scale
        p = masked_softmax(s, valid[:, None])
        return jnp.einsum('bhqk,bqkhd->bqhd', p, vg)

    o = lax.map(one_block, (jnp.arange(nb), qb, qib, wib))
    return o.swapaxes(0, 1).reshape(B_, S_, B_HEADS * HEAD_DIM).astype(v.dtype)


def chunk_band_attention(q, k, v, rel_bias):
    B_, S_ = q.shape[0], q.shape[1]
    nc = S_ // CHUNK
    left = C_LEFT_CHUNKS * CHUNK
    band = left + CHUNK
    kp = jnp.pad(k.astype(jnp.float32), ((0, 0), (left, 0), (0, 0), (0, 0)))
    vp = jnp.pad(v.astype(jnp.float32), ((0, 0), (left, 0), (0, 0), (0, 0)))
    dist = left + jnp.arange(CHUNK)[:, None] - jnp.arange(band)[None, :]
    bias_idx = jnp.clip(dist, -(CHUNK - 1), MAX_REL) + (CHUNK - 1)
    bias = rel_bias.astype(jnp.float32)[:, bias_idx]
    qc = q.reshape(B_, nc, CHUNK, C_HEADS, HEAD_DIM).swapaxes(0, 1)
    scale = HEAD_DIM ** -0.5

    def one_chunk(args):
        ci, qblk = args
        kb = lax.dynamic_slice_in_dim(kp, ci * CHUNK, band, axis=1)
        vb = lax.dynamic_slice_in_dim(vp, ci * CHUNK, band, axis=1)
        valid = (ci * CHUNK - left + jnp.arange(band)) >= 0
        s = jnp.einsum('bqhd,bkhd->bhqk', qblk.astype(jnp.float32), kb) * scale + bias[None]
        p = masked_softmax(s, valid[None, None, None, :])
        return jnp.einsum('bhqk,bkhd->bqhd', p, vb)

    o = lax.map(one_chunk, (jnp.arange(nc), qc))
    return o.swapaxes(0, 1).reshape(B_, S_, C_HEADS * HEAD_DIM).astype(v.dtype)


def even_mixer(h, w_in, w_out, lam_p, subln, rope64, rope32, lambda_init, topk):
    B_, S_ = h.shape[0], h.shape[1]
    cos64, sin64 = rope64
    cos32, sin32 = rope32
    a_q, a_k, a_v, b_q, b_k, b_v, i_q, i_k, i_w = jnp.split(h @ w_in, EVEN_SPLITS, axis=-1)
    a_q = apply_rope(a_q.reshape(B_, S_, 2 * A_HEADS, HEAD_DIM), cos64, sin64).reshape(B_, S_, A_HEADS, 2, HEAD_DIM)
    a_k = apply_rope(a_k.reshape(B_, S_, 2 * A_HEADS, HEAD_DIM), cos64, sin64).reshape(B_, S_, A_HEADS, 2, HEAD_DIM)
    a_v = a_v.reshape(B_, S_, A_HEADS, A_VDIM)
    lp = lam_p.astype(jnp.float32)
    lam = jnp.exp(jnp.sum(lp[0] * lp[1])) - jnp.exp(jnp.sum(lp[2] * lp[3])) + lambda_init
    o_a = diff_attention(a_q, a_k, a_v, lam, subln, lambda_init)
    b_q = apply_rope(b_q.reshape(B_, S_, B_HEADS, HEAD_DIM), cos64, sin64)
    b_k = apply_rope(b_k.reshape(B_, S_, B_HEADS, HEAD_DIM), cos64, sin64)
    b_v = b_v.reshape(B_, S_, B_HEADS, HEAD_DIM)
    i_q = apply_rope(i_q.reshape(B_, S_, IDX_HEADS, IDX_DIM), cos32, sin32)
    i_k = apply_rope(i_k.reshape(B_, S_, 1, IDX_DIM), cos32, sin32)[:, :, 0]
    i_w = i_w * (IDX_HEADS ** -0.5 * IDX_DIM ** -0.5)
    o_b = dsa_attention(b_q, b_k, b_v, i_q, i_k, i_w, topk)
    return jnp.concatenate([o_a, o_b], axis=-1) @ w_out


def odd_mixer(h, w_in, w_out, rel_bias):
    B_, S_ = h.shape[0], h.shape[1]
    q, k, v = jnp.split(h @ w_in, 3, axis=-1)
    q = q.reshape(B_, S_, C_HEADS, HEAD_DIM)
    k = k.reshape(B_, S_, C_HEADS, HEAD_DIM)
    v = v.reshape(B_, S_, C_HEADS, HEAD_DIM)
    return chunk_band_attention(q, k, v, rel_bias) @ w_out


def setup_inputs(seed: int = 0) -> dict:
    key = jax.random.key(seed)
    ks = jax.random.split(key, 12)
    n_even = (DEPTH + 1) // 2
    n_odd = DEPTH // 2
    nrm = jax.random.normal
    f32 = jnp.float32
    return {
        "x": nrm(ks[0], (BATCH, SEQ, D_MODEL), f32),
        "norm_g": 1.0 + 0.01 * nrm(ks[1], (DEPTH, 6, D_MODEL), f32),
        "ffn_wg": nrm(ks[2], (DEPTH, 2, D_MODEL, D_FF), f32) * D_MODEL ** -0.5,
        "ffn_wu": nrm(ks[3], (DEPTH, 2, D_MODEL, D_FF), f32) * D_MODEL ** -0.5,
        "ffn_wd": nrm(ks[4], (DEPTH, 2, D_FF, D_MODEL), f32) * D_FF ** -0.5,
        "even_w_in": nrm(ks[5], (n_even, D_MODEL, EVEN_PROJ), f32) * D_MODEL ** -0.5,
        "even_w_out": nrm(ks[6], (n_even, MIX_WIDTH, D_MODEL), f32) * MIX_WIDTH ** -0.5,
        "even_lambda": 0.1 * nrm(ks[7], (n_even, 4, HEAD_DIM), f32),
        "even_subln": 1.0 + 0.01 * nrm(ks[8], (n_even, A_VDIM), f32),
        "odd_w_in": nrm(ks[9], (n_odd, D_MODEL, 3 * D_MODEL), f32) * D_MODEL ** -0.5,
        "odd_w_out": nrm(ks[10], (n_odd, D_MODEL, D_MODEL), f32) * D_MODEL ** -0.5,
        "odd_rel_bias": 0.1 * nrm(ks[11], (n_odd, C_HEADS, REL_TABLE), f32),
    }


def reference(x, norm_g, ffn_wg, ffn_wu, ffn_wd, even_w_in, even_w_out, even_lambda, even_subln,
              odd_w_in, odd_w_out, odd_rel_bias):
    S_ = x.shape[1]
    topk = min(TOPK_MAX, S_ // 4)
    rope64 = rope_tables(S_, HEAD_DIM)
    rope32 = rope_tables(S_, IDX_DIM)
    for l in range(DEPTH):
        g = norm_g[l]
        h = rmsnorm(x, g[0])
        x = x + 0.5 * rmsnorm(swiglu(h, ffn_wg[l, 0], ffn_wu[l, 0], ffn_wd[l, 0]), g[1])
        h = rmsnorm(x, g[2])
        if l % 2 == 0:
            e = l // 2
            lambda_init = 0.8 - 0.6 * math.exp(-0.3 * l)
            m = even_mixer(h, even_w_in[e], even_w_out[e], even_lambda[e], even_subln[e],
                           rope64, rope32, lambda_init, topk)
        else:
            o = l // 2
            m = odd_mixer(h, odd_w_in[o], odd_w_out[o], odd_rel_bias[o])
        x = x + rmsnorm(m.astype(x.dtype), g[3])
        h = rmsnorm(x, g[4])
        x = x + 0.5 * rmsnorm(swiglu(h, ffn_wg[l, 1], ffn_wu[l, 1], ffn_wd[l, 1]), g[5])
    return x
```

```python
from contextlib import ExitStack
import numpy as np
import concourse.bass as bass
import concourse.mybir as mybir
from concourse.bass_utils import run_bass_kernel_spmd

F32 = mybir.dt.float32
BF16 = mybir.dt.bfloat16
AF = mybir.ActivationFunctionType
ALU = mybir.AluOpType

D = 1024
S = 2048
FF = 2816
NDC = 8
NFC = 22
TG = 512
EPS = 1e-6
N_CORES = 8
SEQ_PER_CORE = 2

ENGS = ("pe", "act", "dve", "pool", "sp")
BLOCKNAME = {"pe": "tensor", "act": "scalar", "dve": "vector", "pool": "gpsimd", "sp": "sync"}


class Op:
    __slots__ = ("eng", "fn", "deps", "is_dma", "semname", "val", "signal", "block")

    def __init__(self, eng, fn, block):
        self.eng = eng
        self.fn = fn
        self.deps = []
        self.is_dma = False
        self.semname = None
        self.val = 0
        self.signal = False
        self.block = block


class Sched:
    def __init__(self, nc, same_eng_sync=True):
        self.nc = nc
        self.same_eng_sync = same_eng_sync
        self.cur = {e: [] for e in ENGS}
        self.block_id = 0
        self.last_w = {}
        self.readers = {}
        self.sems = {}
        self.cnt = {e: 0 for e in ENGS}
        self.dma_cnt = {}
        self.waited = {e: {} for e in ENGS}
        self.stack = ExitStack()
        self.nops = 0

    def sem(self, name):
        if name not in self.sems:
            self.sems[name] = self.stack.enter_context(self.nc.semaphore(name))
        return self.sems[name]

    def add(self, eng, fn, r=(), w=(), dma_key=None):
        op = Op(eng, fn, self.block_id)
        self.nops += 1
        if dma_key is not None:
            op.is_dma = True
            op.semname = "d_" + dma_key
            self.dma_cnt[dma_key] = self.dma_cnt.get(dma_key, 0) + 1
            op.val = 16 * self.dma_cnt[dma_key]
            op.signal = True
            self.sem(op.semname)
        psr = [t for t in r if t.startswith("ps") and t[2:].isdigit()]
        if psr:
            w = list(w) + [t for t in psr if t not in w]
            r = [t for t in r if t not in psr]
        deps = []
        for t in r:
            lw = self.last_w.get(t)
            if lw is not None:
                deps.append(lw)
        for t in w:
            lw = self.last_w.get(t)
            if lw is not None:
                deps.append(lw)
            deps.extend(self.readers.get(t, ()))
        for t in r:
            self.readers.setdefault(t, []).append(op)
        for t in w:
            self.last_w[t] = op
            self.readers[t] = []
        seen = set()
        for d in deps:
            if d is op or id(d) in seen:
                continue
            seen.add(id(d))
            if not d.is_dma:
                if d.block < self.block_id:
                    continue
                if d.eng == eng and not op.is_dma and (not self.same_eng_sync or eng == "pe"):
                    continue
            d.signal = True
            op.deps.append(d)
        self.cur[eng].append(op)
        return op

    def pe(self, fn, r=(), w=()):
        return self.add("pe", fn, r, w)

    def act(self, fn, r=(), w=()):
        return self.add("act", fn, r, w)

    def dve(self, fn, r=(), w=()):
        return self.add("dve", fn, r, w)

    def pool(self, fn, r=(), w=()):
        return self.add("pool", fn, r, w)

    def dma(self, eng, out, in_, key, r=(), w=()):
        return self.add(eng, lambda e: e.dma_start(out=out, in_=in_), r, w, dma_key=key)

    def flush(self):
        cur = self.cur
        if not any(cur[e] for e in ENGS):
            return
        for e in ENGS:
            for op in cur[e]:
                if not op.is_dma and op.signal:
                    self.cnt[e] += 1
                    op.val = self.cnt[e]
                    op.semname = "c_" + e
                    self.sem(op.semname)
        with self.nc.Block() as block:
            for e in ENGS:
                if not cur[e]:
                    continue

                def body(engine, e=e):
                    wt = self.waited[e]
                    for op in cur[e]:
                        for d in sorted(op.deps, key=lambda d: -d.val):
                            if wt.get(d.semname, 0) >= d.val:
                                continue
                            engine.wait_ge(self.sems[d.semname], d.val)
                            wt[d.semname] = d.val
                        ins = op.fn(engine)
                        if op.signal:
                            ins.then_inc(self.sems[op.semname], 16 if op.is_dma else 1)

                getattr(block, BLOCKNAME[e])(body)
        self.cur = {e: [] for e in ENGS}
        self.block_id += 1

    def wait_all_dma(self, eng, ops):
        def fn(engine):
            last = None
            for o in ops:
                last = engine.wait_ge(self.sems[o.semname], o.val)
            return last
        self.cur[eng].append(_mk_plain(eng, fn, self.block_id))


def _mk_plain(eng, fn, block):
    op = Op(eng, fn, block)
    return op


class Ctx:
    pass


_SBT_CNT = [0]


def _sbt(nc, name, shape, dt):
    _SBT_CNT[0] += 1
    return nc.sbuf_tensor(f"{name}_u{_SBT_CNT[0]}", shape, dt)


import os
EVEN_DBG = os.environ.get("EVEN_DBG", "")
LAST_INPUT_NAMES = set()
ALL_STAGES = ("ffn00", "mix0", "ffn01", "ffn10", "mix1", "ffn11")


def build_program(nseq=SEQ_PER_CORE, stages=ALL_STAGES, same_eng_sync=True):
    nc = bass.Bass("TRN2", target_bir_lowering=False)
    c = Ctx()
    c.nc = nc
    c.x = nc.dram_tensor("x", [nseq, S, D], F32, kind="ExternalInput").ap()
    c.y = nc.dram_tensor("y", [nseq, S, D], F32, kind="ExternalOutput").ap()
    c.g_lay = nc.dram_tensor("g_lay", [128, 96], F32, kind="ExternalInput").ap()
    c.ident = nc.dram_tensor("ident", [128, 128], F32, kind="ExternalInput").ap()
    c.wg = nc.dram_tensor("ffn_wg", [2, 2, D, FF], F32, kind="ExternalInput").ap()
    c.wu = nc.dram_tensor("ffn_wu", [2, 2, D, FF], F32, kind="ExternalInput").ap()
    c.wd = nc.dram_tensor("ffn_wd", [2, 2, FF, D], F32, kind="ExternalInput").ap()

    c.e_w_in = nc.dram_tensor("even_w_in", [1, D, 3368], F32, kind="ExternalInput").ap()
    c.e_w_out = nc.dram_tensor("even_w_out", [1, D, D], F32, kind="ExternalInput").ap()
    c.o_w_in = nc.dram_tensor("odd_w_in", [1, D, 3 * D], F32, kind="ExternalInput").ap()
    c.o_w_out = nc.dram_tensor("odd_w_out", [1, D, D], F32, kind="ExternalInput").ap()
    c.biasT = nc.dram_tensor("odd_biasT", [16, 128, 640], F32, kind="ExternalInput").ap()
    c.bmask = nc.dram_tensor("odd_mask", [128, 640], F32, kind="ExternalInput").ap()
    c.rope = nc.dram_tensor("rope_tabs", [4, 128, S], F32, kind="ExternalInput").ap()
    c.rmat = nc.dram_tensor("rot_mats", [2, 128, 128], F32, kind="ExternalInput").ap()
    c.lam_lay = nc.dram_tensor("lam_lay", [128, 256], F32, kind="ExternalInput").ap()
    c.subln_lay = nc.dram_tensor("subln_lay", [128, 1], F32, kind="ExternalInput").ap()
    LAST_INPUT_NAMES.clear()
    LAST_INPUT_NAMES.update(["x", "g_lay", "ident", "ffn_wg", "ffn_wu", "ffn_wd", "even_w_in", "even_w_out",
                             "odd_w_in", "odd_w_out", "odd_biasT", "odd_mask", "rope_tabs", "rot_mats",
                             "lam_lay", "subln_lay"])
    sch = Sched(nc, same_eng_sync=same_eng_sync)
    c.s = sch
    es = ExitStack()
    with es, sch.stack:
        def sb(name, shape, dt):
            return es.enter_context(_sbt(nc, name, shape, dt))

        def ps(name, shape, dt):
            return es.enter_context(nc.psum_tensor(name, shape, dt))

        c.xT = sb("xT", [128, NDC, S], F32)
        c.g = sb("g", [128, 96], F32)
        c.gh = sb("gh", [128, 96], F32)
        c.identf = sb("identf", [128, 128], F32)
        c.ones = sb("ones", [128, 128], F32)
        c.epsb = sb("epsb", [128, 1], F32)
        c.identb = sb("identb", [128, 128], BF16)
        c.onesb = sb("onesb", [128, 128], BF16)
        c.P = ps("psall", [128, 8 * TG], F32)
        c.psum = [c.P[:, i * TG:(i + 1) * TG] for i in range(8)]

        sch.dma("sp", c.g[:], c.g_lay[:, :], "c0", w=["g"])
        sch.dma("sp", c.identf[:], c.ident[:, :], "c1", w=["ident"])
        sch.dve(lambda e: e.memset(c.ones[:], 1.0), w=["ones"])
        sch.dve(lambda e: e.memset(c.epsb[:], EPS), w=["epsb"])
        sch.dve(lambda e: e.memset(c.onesb[:], 1.0), w=["onesb"])
        sch.dve(lambda e: e.tensor_copy(out=c.identb[:], in_=c.identf[:]), r=["ident"], w=["identb"])
        sch.dve(lambda e: e.tensor_scalar_mul(c.gh[:], c.g[:], 0.5), r=["g"], w=["gh"])
        sch.flush()

        for sq in range(nseq):
            c.xspill = c.y[sq].rearrange("(p a) d -> p (a d)", p=128)
            load_x(c, sq)
            sch.flush()
            for st in stages:
                if st.startswith("ffn"):
                    l, i = int(st[3]), int(st[4])
                    ffn(c, l, i)
                elif st == "mix0":
                    mixer_even(c)
                elif st == "mix1":
                    mixer_odd(c)
                sch.flush()
            store_x(c, sq)
            sch.flush()
    return nc


def load_x(c, sq):
    nc, s = c.nc, c.s
    with ExitStack() as es:
        xin = [es.enter_context(_sbt(nc, f"xin{i}", [128, D], F32)) for i in range(2)]
        for tt in range(S // 128):
            b = tt % 2
            s.dma("sp", xin[b][:], c.x[sq, tt * 128:(tt + 1) * 128, :], f"xin{b}", w=[f"xin{b}"])
            for half in range(2):
                bank = c.psum[(2 * tt + half) % 4]
                bk = f"ps{(2 * tt + half) % 4}"

                def tr(e, bank=bank, b=b, half=half):
                    last = None
                    for j in range(4):
                        dc = half * 4 + j
                        last = e.transpose(bank[:, j * 128:(j + 1) * 128], xin[b][:, dc * 128:(dc + 1) * 128], c.identf[:])
                    return last
                s.pe(tr, r=[f"xin{b}", "ident"], w=[bk])
                dst = c.xT[:, half * 4:half * 4 + 4, tt * 128:(tt + 1) * 128]
                src = bank[:].rearrange("p (j t) -> p j t", j=4)
                if half == 0:
                    s.act(lambda e, dst=dst, src=src: e.copy(out=dst, in_=src), r=[bk], w=[f"xl:{tt}:a"])
                else:
                    s.dve(lambda e, dst=dst, src=src: e.tensor_copy(out=dst, in_=src), r=[bk], w=[f"xl:{tt}:b"])
        s.flush()


def store_x(c, sq):
    nc, s = c.nc, c.s
    with ExitStack() as es:
        xo = [es.enter_context(_sbt(nc, f"xo{i}", [128, D], F32)) for i in range(2)]
        outs = []
        for tt in range(S // 128):
            b = tt % 2
            for half in range(2):
                bank = c.psum[(2 * tt + half) % 4]
                bk = f"ps{(2 * tt + half) % 4}"

                def tr(e, bank=bank, half=half, tt=tt):
                    last = None
                    for j in range(4):
                        dc = half * 4 + j
                        last = e.transpose(bank[:, j * 128:(j + 1) * 128], c.xT[:, dc, tt * 128:(tt + 1) * 128], c.identf[:])
                    return last
                s.pe(tr, r=[f"x:{tt // 4}", "ident"], w=[bk])
                dst = xo[b][:, half * 512:(half + 1) * 512]
                if half == 0:
                    s.act(lambda e, dst=dst, bank=bank: e.copy(out=dst, in_=bank[:]), r=[bk], w=[f"xo{b}"])
                else:
                    s.dve(lambda e, dst=dst, bank=bank: e.tensor_copy(out=dst, in_=bank[:]), r=[bk], w=[f"xo{b}"])
            outs.append(s.dma("sp", c.y[sq, tt * 128:(tt + 1) * 128, :], xo[b][:], f"xo{b}", r=[f"xo{b}"]))
        s.wait_all_dma("sp", outs[-2:])
        s.flush()


def rms_rstd(c, ss_bank, bk, tmp, rstd, tok_rstd):
    s = c.s
    s.act(lambda e: e.activation(out=tmp[:], in_=ss_bank[:], func=AF.Sqrt, scale=1.0 / D, bias=c.epsb[:]),
          r=[bk, "epsb"], w=["rtmp"])
    s.dve(lambda e: e.reciprocal(out=rstd[:], in_=tmp[:]), r=["rtmp"], w=[tok_rstd])


def ffn(c, l, i):
    nc, s = c.nc, c.s
    gi0 = (l * 6 + (0 if i == 0 else 4)) * 8
    gi1 = (l * 6 + (1 if i == 0 else 5)) * 8
    wg = c.wg[l, i].rearrange("(c p) f -> p c f", p=128)
    wu = c.wu[l, i].rearrange("(c p) f -> p c f", p=128)
    wd = c.wd[l, i].rearrange("(k p) d -> p k d", p=128)
    NW = 3
    with ExitStack() as es:
        def sb(name, shape, dt):
            return es.enter_context(_sbt(nc, name, shape, dt))
        hT = sb("f_hT", [128, NDC, 1024], BF16)
        AT = sb("f_AT", [128, NFC, 1024], BF16)
        Yb = sb("f_Y", [128, NDC, 1024], F32)
        wgs = [sb(f"f_wg{j}", [128, NDC, 256], BF16) for j in range(NW)]
        wus = [sb(f"f_wu{j}", [128, NDC, 256], BF16) for j in range(NW)]
        wds = [sb(f"f_wd{j}", [128, NFC, 128], BF16) for j in range(2)]
        sq = [sb(f"f_sq{j}", [128, TG], F32) for j in range(2)]
        sg = [sb(f"f_sg{j}", [128, TG], F32) for j in range(2)]
        rtmp = sb("f_rtmp", [128, TG], F32)
        rstd = [sb(f"f_rstd{j}", [128, TG], F32) for j in range(2)]

        for hf in range(2):
            t0 = hf * 1024
            for tgl in range(2):
                tok = t0 + tgl * TG
                xg = f"x:{tok // TG}"
                bank = c.psum[6 + tgl]
                bk = f"ps{6 + tgl}"
                for dc in range(NDC):
                    j = dc % 2
                    s.act(lambda e, j=j, dc=dc, tok=tok: e.activation(out=sq[j][:], in_=c.xT[:, dc, tok:tok + TG], func=AF.Square),
                          r=[xg], w=[f"sq{j}"])
                    s.pe(lambda e, j=j, dc=dc, bank=bank: e.matmul(bank[:], c.ones[:], sq[j][:], start=(dc == 0), stop=(dc == NDC - 1)),
                         r=[f"sq{j}", "ones"], w=[bk])
                rms_rstd(c, bank, bk, rtmp, rstd[tgl], f"rstd{tgl}")
                for dc in range(NDC):
                    s.dve(lambda e, dc=dc, tok=tok, tgl=tgl: e.scalar_tensor_tensor(
                        out=hT[:, dc, tgl * TG:(tgl + 1) * TG], in0=c.xT[:, dc, tok:tok + TG],
                        scalar=c.g[:, gi0 + dc:gi0 + dc + 1], in1=rstd[tgl][:], op0=ALU.mult, op1=ALU.mult),
                        r=[xg, f"rstd{tgl}", "g"], w=[f"h:{tgl}"])
            k = 0
            for gq in range(NFC // 2):
                slot = gq % NW
                s.dma("pool", wgs[slot][:], wg[:, :, gq * 256:(gq + 1) * 256], f"wg{slot}", w=[f"wg{slot}"])
                s.dma("pool", wus[slot][:], wu[:, :, gq * 256:(gq + 1) * 256], f"wu{slot}", w=[f"wu{slot}"])
                for j in range(2):
                    ffc = 2 * gq + j
                    for tgl in range(2):
                        bG, bU = c.psum[2 * (k % 2)], c.psum[2 * (k % 2) + 1]
                        kG, kU = f"ps{2 * (k % 2)}", f"ps{2 * (k % 2) + 1}"
                        sgk = sg[k % 2]

                        def mmg(e, W, bank, j=j, tgl=tgl):
                            last = None
                            for dc in range(NDC):
                                last = e.matmul(bank[:], W[:, dc, j * 128:(j + 1) * 128], hT[:, dc, tgl * TG:(tgl + 1) * TG],
                                                start=(dc == 0), stop=(dc == NDC - 1))
                            return last
                        s.pe(lambda e, W=wgs[slot], bank=bG, f=mmg: f(e, W, bank), r=[f"wg{slot}", f"h:{tgl}"], w=[kG])
                        s.pe(lambda e, W=wus[slot], bank=bU, f=mmg: f(e, W, bank), r=[f"wu{slot}", f"h:{tgl}"], w=[kU])
                        s.act(lambda e, sgk=sgk, bG=bG: e.activation(out=sgk[:], in_=bG[:], func=AF.Silu), r=[kG], w=[f"sg{k % 2}"])
                        s.dve(lambda e, sgk=sgk, bU=bU, ffc=ffc, tgl=tgl: e.tensor_tensor(
                            out=AT[:, ffc, tgl * TG:(tgl + 1) * TG], in0=sgk[:], in1=bU[:], op=ALU.mult),
                            r=[kU, f"sg{k % 2}"], w=[f"AT:{tgl}"])
                        k += 1
            pend = None
            k = 0
            for dc in range(NDC):
                slot = dc % 2
                s.dma("pool", wds[slot][:], wd[:, :, dc * 128:(dc + 1) * 128], f"wd{slot}", w=[f"wd{slot}"])
                for tgl in range(2):
                    bY = c.psum[4 + (k % 2)]
                    kY = f"ps{4 + (k % 2)}"

                    def mmy(e, W=wds[slot], bank=bY, tgl=tgl):
                        last = None
                        for kc in range(NFC):
                            last = e.matmul(bank[:], W[:, kc, :], AT[:, kc, tgl * TG:(tgl + 1) * TG],
                                            start=(kc == 0), stop=(kc == NFC - 1))
                        return last
                    s.pe(mmy, r=[f"wd{slot}", f"AT:{tgl}"], w=[kY])
                    if pend is not None:
                        pend()
                    sqk = sq[k % 2]
                    s.act(lambda e, bY=bY, dc=dc, tgl=tgl: e.copy(out=Yb[:, dc, tgl * TG:(tgl + 1) * TG], in_=bY[:]),
                          r=[kY], w=[f"Y:{tgl}"])
                    s.act(lambda e, bY=bY, sqk=sqk: e.activation(out=sqk[:], in_=bY[:], func=AF.Square),
                          r=[kY], w=[f"sq{k % 2}"])

                    def ssmm(sqk=sqk, k=k, dc=dc, tgl=tgl):
                        bank = c.psum[6 + tgl]
                        s.pe(lambda e: e.matmul(bank[:], c.ones[:], sqk[:], start=(dc == 0), stop=(dc == NDC - 1)),
                             r=[f"sq{k % 2}", "ones"], w=[f"ps{6 + tgl}"])
                    pend = ssmm
                    k += 1
            pend()
            for tgl in range(2):
                tok = t0 + tgl * TG
                xg = f"x:{tok // TG}"
                rms_rstd(c, c.psum[6 + tgl], f"ps{6 + tgl}", rtmp, rstd[tgl], f"rstd{tgl}")
                for dc in range(NDC):
                    ysl = Yb[:, dc, tgl * TG:(tgl + 1) * TG]
                    s.dve(lambda e, ysl=ysl, tgl=tgl: e.tensor_tensor(out=ysl, in0=ysl, in1=rstd[tgl][:], op=ALU.mult),
                          r=[f"Y:{tgl}", f"rstd{tgl}"], w=[f"Y:{tgl}"])
                    xs = c.xT[:, dc, tok:tok + TG]
                    s.dve(lambda e, ysl=ysl, xs=xs, dc=dc: e.scalar_tensor_tensor(
                        out=xs, in0=ysl, scalar=c.gh[:, gi1 + dc:gi1 + dc + 1], in1=xs, op0=ALU.mult, op1=ALU.add),
                        r=[f"Y:{tgl}", "gh", xg], w=[xg])
        s.flush()


def rms_hT(c, hT, gi):
    nc, s = c.nc, c.s
    with ExitStack() as es:
        sq = [es.enter_context(_sbt(nc, f"r_sq{j}", [128, TG], F32)) for j in range(2)]
        rtmp = es.enter_context(_sbt(nc, "r_rtmp", [128, TG], F32))
        rstd = [es.enter_context(_sbt(nc, f"r_rstd{j}", [128, TG], F32)) for j in range(2)]
        for tg in range(4):
            tok = tg * TG
            xg = f"x:{tg}"
            bank = c.psum[6 + tg % 2]
            bk = f"ps{6 + tg % 2}"
            for dc in range(NDC):
                j = dc % 2
                s.act(lambda e, j=j, dc=dc, tok=tok: e.activation(out=sq[j][:], in_=c.xT[:, dc, tok:tok + TG], func=AF.Square),
                      r=[xg], w=[f"sq{j}"])
                s.pe(lambda e, j=j, dc=dc, bank=bank: e.matmul(bank[:], c.ones[:], sq[j][:], start=(dc == 0), stop=(dc == NDC - 1)),
                     r=[f"sq{j}", "ones"], w=[bk])
            rms_rstd(c, bank, bk, rtmp, rstd[tg % 2], f"rstd{tg % 2}")
            for dc in range(NDC):
                s.dve(lambda e, dc=dc, tok=tok, tg=tg: e.scalar_tensor_tensor(
                    out=hT[:, dc, tok:tok + TG], in0=c.xT[:, dc, tok:tok + TG],
                    scalar=c.g[:, gi + dc:gi + dc + 1], in1=rstd[tg % 2][:], op0=ALU.mult, op1=ALU.mult),
                    r=[xg, f"rstd{tg % 2}", "g"], w=[f"h:{tg}"])
        s.flush()


def out_proj(c, OT, W, gi):
    nc, s = c.nc, c.s
    Wv = W.rearrange("(k p) d -> p k d", p=128)
    with ExitStack() as es:
        def sb(name, shape, dt):
            return es.enter_context(_sbt(nc, name, shape, dt))
        Wb = sb("p_W", [128, NDC, D], BF16)
        Yb = sb("p_Y", [128, NDC, 1024], F32)
        sq = [sb(f"p_sq{j}", [128, TG], F32) for j in range(2)]
        rtmp = sb("p_rtmp", [128, TG], F32)
        rstd = [sb(f"p_rstd{j}", [128, TG], F32) for j in range(2)]
        s.dma("pool", Wb[:, 0:4, :], Wv[:, 0:4, :], "pw0", w=["pW0"])
        s.dma("pool", Wb[:, 4:8, :], Wv[:, 4:8, :], "pw1", w=["pW1"])
        for hf in range(2):
            pend = None
            k = 0
            for dc in range(NDC):
                for tgl in range(2):
                    tg = hf * 2 + tgl
                    bY = c.psum[4 + (k % 2)]
                    kY = f"ps{4 + (k % 2)}"

                    def mmy(e, bank=bY, tg=tg, dc=dc):
                        last = None
                        for kc in range(NDC):
                            last = e.matmul(bank[:], Wb[:, kc, dc * 128:(dc + 1) * 128], OT[:, kc, tg * TG:(tg + 1) * TG],
                                            start=(kc == 0), stop=(kc == NDC - 1))
                        return last
                    s.pe(mmy, r=["pW0", "pW1", "OT"], w=[kY])
                    if pend is not None:
                        pend()
                    sqk = sq[k % 2]
                    s.act(lambda e, bY=bY, dc=dc, tgl=tgl: e.copy(out=Yb[:, dc, tgl * TG:(tgl + 1) * TG], in_=bY[:]),
                          r=[kY], w=[f"Y:{tgl}"])
                    s.act(lambda e, bY=bY, sqk=sqk: e.activation(out=sqk[:], in_=bY[:], func=AF.Square),
                          r=[kY], w=[f"sq{k % 2}"])

                    def ssmm(sqk=sqk, k=k, dc=dc, tgl=tgl):
                        bank = c.psum[6 + tgl]
                        s.pe(lambda e: e.matmul(bank[:], c.ones[:], sqk[:], start=(dc == 0), stop=(dc == NDC - 1)),
                             r=[f"sq{k % 2}", "ones"], w=[f"ps{6 + tgl}"])
                    pend = ssmm
                    k += 1
            pend()
            for tgl in range(2):
                tg = hf * 2 + tgl
                tok = tg * TG
                xg = f"x:{tg}"
                rms_rstd(c, c.psum[6 + tgl], f"ps{6 + tgl}", rtmp, rstd[tgl], f"rstd{tgl}")
                for dc in range(NDC):
                    ysl = Yb[:, dc, tgl * TG:(tgl + 1) * TG]
                    s.dve(lambda e, ysl=ysl, tgl=tgl: e.tensor_tensor(out=ysl, in0=ysl, in1=rstd[tgl][:], op=ALU.mult),
                          r=[f"Y:{tgl}", f"rstd{tgl}"], w=[f"Y:{tgl}"])
                    xs = c.xT[:, dc, tok:tok + TG]
                    s.dve(lambda e, ysl=ysl, xs=xs, dc=dc: e.scalar_tensor_tensor(
                        out=xs, in0=ysl, scalar=c.g[:, gi + dc:gi + dc + 1], in1=xs, op0=ALU.mult, op1=ALU.add),
                        r=[f"Y:{tgl}", "g", xg], w=[xg])
        s.flush()


def mixer_odd(c):
    nc, s = c.nc, c.s
    gi2, gi3 = (6 + 2) * 8, (6 + 3) * 8
    w_in = c.o_w_in[0].rearrange("(c p) f -> p c f", p=128)
    with ExitStack() as eo:
        OT = eo.enter_context(_sbt(nc, "o_OT", [128, NDC, S], BF16))
        with ExitStack() as es:
            def sb(name, shape, dt):
                return es.enter_context(_sbt(nc, name, shape, dt))
            hT = sb("o_hT", [128, NDC, S], BF16)
            EB = sb("o_EB", [128, 16, 640], BF16)
            rms_hT(c, hT, gi2)
            with ExitStack() as e2:
                bt = [e2.enter_context(_sbt(nc, f"o_bt{j}", [128, 640], F32)) for j in range(2)]
                mk = e2.enter_context(_sbt(nc, "o_mk", [128, 640], F32))
                s.dma("sp", mk[:], c.bmask[:, :], "omk", w=["omk"])
                for h in range(16):
                    j = h % 2
                    s.dma("sp", bt[j][:], c.biasT[h], f"obt{j}", w=[f"obt{j}"])
                    s.act(lambda e, j=j: e.activation(out=bt[j][:], in_=bt[j][:], func=AF.Exp), r=[f"obt{j}"], w=[f"obt{j}"])
                    s.dve(lambda e, j=j, h=h: e.tensor_tensor(out=EB[:, h, :], in0=bt[j][:], in1=mk[:], op=ALU.mult),
                          r=[f"obt{j}", "omk"], w=["EB"])
                s.flush()
            Wq = [sb(f"o_wq{j}", [128, NDC, 128], BF16) for j in range(2)]
            Wk = [sb(f"o_wk{j}", [128, NDC, 128], BF16) for j in range(2)]
            Wv = [sb(f"o_wv{j}", [128, NDC, 128], BF16) for j in range(2)]
            QT = sb("o_QT", [128, S], BF16)
            KT = sb("o_KT", [128, S], BF16)
            V = sb("o_V", [128, 16, 128], BF16)
            PR = [sb(f"o_pr{j}", [128, 640], BF16) for j in range(2)]
            PT = [sb(f"o_pt{j}", [128, 640], BF16) for j in range(2)]
            rec = [sb(f"o_rec{j}", [128, 128], F32) for j in range(2)]
            pc = 0
            for hp in range(8):
                sl = hp % 2
                s.dma("pool", Wq[sl][:], w_in[:, :, hp * 128:(hp + 1) * 128], f"owq{sl}", w=[f"owq{sl}"])
                s.dma("pool", Wk[sl][:], w_in[:, :, D + hp * 128:D + (hp + 1) * 128], f"owk{sl}", w=[f"owk{sl}"])
                s.dma("pool", Wv[sl][:], w_in[:, :, 2 * D + hp * 128:2 * D + (hp + 1) * 128], f"owv{sl}", w=[f"owv{sl}"])
                for (W, dst, wtok, dtok) in ((Wq[sl], QT, f"owq{sl}", "oQ"), (Wk[sl], KT, f"owk{sl}", "oK")):
                    for tg in range(4):
                        bank = c.psum[pc % 2]
                        bk = f"ps{pc % 2}"
                        pc += 1

                        def mm(e, W=W, bank=bank, tg=tg):
                            last = None
                            for dc in range(NDC):
                                last = e.matmul(bank[:], W[:, dc, :], hT[:, dc, tg * TG:(tg + 1) * TG],
                                                start=(dc == 0), stop=(dc == NDC - 1))
                            return last
                        s.pe(mm, r=[wtok, f"h:{tg}"], w=[bk])
                        s.act(lambda e, dst=dst, bank=bank, tg=tg: e.copy(out=dst[:, tg * TG:(tg + 1) * TG], in_=bank[:]),
                              r=[bk], w=[dtok])
                for t4 in range(4):
                    bank = c.psum[2 + t4 % 2]
                    bk = f"ps{2 + t4 % 2}"

                    def mmv(e, bank=bank, t4=t4, W=Wv[sl]):
                        last = None
                        for j in range(4):
                            tt = t4 * 4 + j
                            for dc in range(NDC):
                                last = e.matmul(bank[:, j * 128:(j + 1) * 128], hT[:, dc, tt * 128:(tt + 1) * 128], W[:, dc, :],
                                                start=(dc == 0), stop=(dc == NDC - 1))
                        return last
                    s.pe(mmv, r=[f"owv{sl}", f"h:{t4}"], w=[bk])
                    s.dve(lambda e, bank=bank, t4=t4: e.tensor_copy(out=V[:, t4 * 4:(t4 + 1) * 4, :],
                                                                   in_=bank[:].rearrange("p (a b) -> p a b", a=4)),
                          r=[bk], w=["oV"])
                items = [(b, hh) for b in range(16) for hh in range(2)]

                def kts_of(b):
                    return [kt for kt in range(b - 4, b + 1) if kt >= 0]

                def emit_S(i):
                    b, hh = items[i]
                    sb0 = 2 * (i % 2)

                    def fn(e):
                        last = None
                        for kt in kts_of(b):
                            col = sb0 * TG + (b - kt) * 128
                            last = e.matmul(c.P[:, col:col + 128], KT[hh * 64:(hh + 1) * 64, kt * 128:(kt + 1) * 128],
                                            QT[hh * 64:(hh + 1) * 64, b * 128:(b + 1) * 128], start=True, stop=True)
                        return last
                    s.pe(fn, r=["oQ", "oK"], w=[f"ps{sb0}", f"ps{sb0 + 1}"])

                def emit_rest(i):
                    b, hh = items[i]
                    h = 2 * hp + hh
                    sb0 = 2 * (i % 2)
                    kts = kts_of(b)
                    ncols = len(kts) * 128
                    j = i % 2
                    s.act(lambda e: e.activation(out=PR[j][:, 0:ncols], in_=c.P[:, sb0 * TG:sb0 * TG + ncols], func=AF.Exp, scale=0.125),
                          r=[f"ps{sb0}", f"ps{sb0 + 1}"], w=[f"opr{j}"])
                    s.dve(lambda e: e.tensor_tensor(out=PT[j][:, 0:ncols], in0=PR[j][:, 0:ncols], in1=EB[:, h, 0:ncols], op=ALU.mult),
                          r=[f"opr{j}", "EB"], w=[f"opt{j}"])
                    if i + 1 < len(items):
                        emit_S(i + 1)
                    ob = 4 + 2 * (b % 2)

                    def fn(e):
                        last = None
                        n = len(kts)
                        for idx, kt in enumerate(kts):
                            r_ = b - kt
                            e.matmul(c.psum[ob][hh * 64:(hh + 1) * 64, 0:128], V[:, kt, hh * 64:(hh + 1) * 64],
                                     PT[j][:, r_ * 128:(r_ + 1) * 128], start=(idx == 0), stop=(idx == n - 1))
                        for idx, kt in enumerate(kts):
                            r_ = b - kt
                            last = e.matmul(c.psum[ob + 1][hh * 64:(hh + 1) * 64, 0:128], c.onesb[:, 0:64],
                                            PT[j][:, r_ * 128:(r_ + 1) * 128], start=(idx == 0), stop=(idx == n - 1))
                        return last
                    s.pe(fn, r=[f"opt{j}", "oV", "onesb"], w=[f"ps{ob}", f"ps{ob + 1}"])
                    if hh == 1:
                        rj = b % 2
                        s.dve(lambda e: e.reciprocal(out=rec[rj][:], in_=c.psum[ob + 1][:, 0:128]), r=[f"ps{ob + 1}"], w=[f"orec{rj}"])
                        s.dve(lambda e: e.tensor_tensor(out=OT[:, hp, b * 128:(b + 1) * 128], in0=c.psum[ob][:, 0:128],
                                                        in1=rec[rj][:], op=ALU.mult),
                              r=[f"ps{ob}", f"orec{rj}"], w=["OT"])
                emit_S(0)
                for i in range(len(items)):
                    emit_rest(i)
                s.flush()
        out_proj(c, OT, c.o_w_out[0], gi3)


def mixer_even(c):
    nc, s = c.nc, c.s
    gi2, gi3 = 2 * 8, 3 * 8
    LAM_INIT = 0.8 - 0.6 * 1.0
    w_in = c.e_w_in[0].rearrange("(c p) f -> p c f", p=128)
    NEG = -1.0e30
    with ExitStack() as eo:
        def sbo(name, shape, dt):
            return eo.enter_context(_sbt(nc, name, shape, dt))
        OT = sbo("e_OT", [128, NDC, S], BF16)
        qiT = sbo("e_qiT", [128, 2, S], BF16)
        kiT4 = sbo("e_kiT", [128, S], BF16)
        wq = sbo("e_wq", [128, 128], F32)
        Bqv = c.xT[:, 0:2, :].bitcast(BF16)
        Bkv = c.xT[:, 2:4, :].bitcast(BF16)
        Bvv = c.xT[:, 4:6, :].bitcast(BF16)

        def BqT(j):
            return Bqv[:, j // 2, (j % 2) * S:(j % 2 + 1) * S]

        def BkT(j):
            return Bkv[:, j // 2, (j % 2) * S:(j % 2 + 1) * S]

        def Bv(kt):
            return Bvv[:, kt // 8, (kt % 8) * 512:(kt % 8 + 1) * 512]
        acc = c.xT[:, 6, :]
        work = c.xT[:, 7, :]
        with ExitStack() as es:
            def sb(name, shape, dt):
                return es.enter_context(_sbt(nc, name, shape, dt))
            hT = sb("e_hT", [128, NDC, S], BF16)
            rms_hT(c, hT, gi2)
            spill = s.dma("sp", c.xspill[:, :], c.xT[:].rearrange("p a b -> p (a b)"), "xsp", r=[f"x:{tg}" for tg in range(4)], w=["xspill"])
            s.wait_all_dma("sp", [spill])
            s.flush()
            tabs = [sb(f"e_tab{j}", [128, S], BF16) for j in range(4)]
            Rm = [sb(f"e_rm{j}", [128, 128], BF16) for j in range(2)]
            rmf = sb("e_rmf", [128, 128], F32)
            lam = sb("e_lam", [128, 256], F32)
            sc = sb("e_sc", [128, 8], F32)
            W = [sb(f"e_w{j}", [128, NDC, 128], BF16) for j in range(3)]
            Xb = sb("e_xb", [128, TG], BF16)
            t1 = sb("e_t1", [128, TG], F32)
            t2 = sb("e_t2", [128, TG], F32)
            for j in range(4):
                s.dma("pool", tabs[j][:], c.rope[j], f"etab{j}", w=[f"tab{j}"])
            for j in range(2):
                s.dma("sp", rmf[:], c.rmat[j], "ermf", w=["rmf"])
                s.dve(lambda e, j=j: e.tensor_copy(out=Rm[j][:], in_=rmf[:]), r=["rmf"], w=[f"rm{j}"])
            s.dma("sp", lam[:], c.lam_lay[:, :], "elam", w=["lam"])
            s.dma("sp", sc[:, 4:5], c.subln_lay[:, :], "esub", w=["sc4"])
            s.dve(lambda e: e.tensor_tensor(out=lam[:, 0:64], in0=lam[:, 0:64], in1=lam[:, 64:128], op=ALU.mult), r=["lam"], w=["lam"])
            s.dve(lambda e: e.tensor_tensor(out=lam[:, 128:192], in0=lam[:, 128:192], in1=lam[:, 192:256], op=ALU.mult), r=["lam"], w=["lam"])
            s.dve(lambda e: e.reduce_sum(out=sc[:, 0:1], in_=lam[:, 0:64], axis=mybir.AxisListType.X), r=["lam"], w=["sc"])
            s.dve(lambda e: e.reduce_sum(out=sc[:, 1:2], in_=lam[:, 128:192], axis=mybir.AxisListType.X), r=["lam"], w=["sc"])
            s.act(lambda e: e.activation(out=sc[:, 0:2], in_=sc[:, 0:2], func=AF.Exp), r=["sc"], w=["sc"])
            s.dve(lambda e: e.tensor_tensor(out=sc[:, 2:3], in0=sc[:, 1:2], in1=sc[:, 0:1], op=ALU.subtract), r=["sc"], w=["sc"])
            s.dve(lambda e: e.tensor_scalar_add(sc[:, 2:3], sc[:, 2:3], -LAM_INIT), r=["sc"], w=["sc"])
            s.dve(lambda e: e.tensor_scalar_mul(sc[:, 3:4], sc[:, 4:5], 1.0 - LAM_INIT), r=["sc", "sc4"], w=["sc"])
            s.flush()

            wcnt = [0]

            def load_w(col0, ncol=128, dst_col=0, slot=None):
                if slot is None:
                    slot = wcnt[0] % 3
                    wcnt[0] += 1
                s.dma("pool", W[slot][:, :, dst_col:dst_col + ncol], w_in[:, :, col0:col0 + ncol], f"ew{slot}", w=[f"ew{slot}"])
                return W[slot], f"ew{slot}", slot

            def rope_proj(Wt, wtok, dst_fn, dtok, ti, rmi):
                for tg in range(4):
                    b6, b7 = c.psum[6], c.psum[7]

                    def mm(e, tg=tg):
                        last = None
                        for dc in range(NDC):
                            last = e.matmul(b6[:], Wt[:, dc, :], hT[:, dc, tg * TG:(tg + 1) * TG], start=(dc == 0), stop=(dc == NDC - 1))
                        return last
                    s.pe(mm, r=(list(wtok) if isinstance(wtok, (list, tuple)) else [wtok]) + [f"h:{tg}"], w=["ps6"])
                    s.act(lambda e: e.copy(out=Xb[:], in_=b6[:]), r=["ps6"], w=["xb"])
                    s.pe(lambda e: e.matmul(b7[:], Rm[rmi][:], Xb[:], start=True, stop=True), r=["xb", f"rm{rmi}"], w=["ps7"])
                    s.dve(lambda e, tg=tg: e.tensor_tensor(out=t1[:], in0=b6[:], in1=tabs[ti][:, tg * TG:(tg + 1) * TG], op=ALU.mult),
                          r=["ps6", f"tab{ti}"], w=["t1"])
                    s.dve(lambda e, tg=tg: e.tensor_tensor(out=t2[:], in0=b7[:], in1=tabs[ti + 1][:, tg * TG:(tg + 1) * TG], op=ALU.mult),
                          r=["ps7", f"tab{ti + 1}"], w=["t2"])
                    s.pool(lambda e, tg=tg: e.tensor_tensor(out=dst_fn(tg), in0=t1[:], in1=t2[:], op=ALU.add),
                           r=["t1", "t2"], w=[dtok])

            with ExitStack() as ea:
                def sba(name, shape, dt):
                    return ea.enter_context(_sbt(nc, name, shape, dt))
                AQ = sba("a_Q", [128, S], BF16)
                AK = sba("a_K", [128, S], BF16)
                AV = sba("a_V", [128, 16, 128], BF16)
                PTa = [sba(f"a_pt{j}", [128, S], BF16) for j in range(2)]
                Ob = [sba(f"a_ob{j}", [128, 4, 256], F32) for j in range(2)]
                tA = sba("a_tA", [128, 4, 128], F32)
                tB = sba("a_tB", [128, 4, 128], F32)
                for h in range(4):
                    Wt, wtok, _ = load_w(h * 128)
                    rope_proj(Wt, wtok, lambda tg: AQ[:, tg * TG:(tg + 1) * TG], "aQ", 0, 0)
                    Wt, wtok, _ = load_w(512 + h * 128)
                    rope_proj(Wt, wtok, lambda tg: AK[:, tg * TG:(tg + 1) * TG], "aK", 0, 0)
                    Wt, wtok, _ = load_w(1024 + h * 128)
                    for t4 in range(4):
                        bank = c.psum[4 + t4 % 2]
                        bk = f"ps{4 + t4 % 2}"

                        def mmv(e, bank=bank, t4=t4, Wt=Wt):
                            last = None
                            for j in range(4):
                                tt = t4 * 4 + j
                                for dc in range(NDC):
                                    last = e.matmul(bank[:, j * 128:(j + 1) * 128], hT[:, dc, tt * 128:(tt + 1) * 128], Wt[:, dc, :],
                                                    start=(dc == 0), stop=(dc == NDC - 1))
                            return last
                        s.pe(mmv, r=[wtok, f"h:{t4}"], w=[bk])
                        s.dve(lambda e, bank=bank, t4=t4: e.tensor_copy(out=AV[:, t4 * 4:(t4 + 1) * 4, :],
                                                                       in_=bank[:].rearrange("p (a b) -> p a b", a=4)),
                              r=[bk], w=["aV"])
                    items = [(tg, comp, bl) for tg in range(4) for comp in range(2) for bl in range(4)]
                    gcount = [0]
                    sbanks = {}

                    def emit_S(i):
                        tg, comp, bl = items[i]
                        b = tg * 4 + bl
                        groups = []
                        for g0 in range(0, b + 1, 4):
                            kts = list(range(g0, min(g0 + 4, b + 1)))
                            bi = gcount[0] % 4
                            gcount[0] += 1

                            def fn(e, kts=kts, bi=bi):
                                last = None
                                for j, kt in enumerate(kts):
                                    last = e.matmul(c.psum[bi][:, j * 128:(j + 1) * 128],
                                                    AK[comp * 64:(comp + 1) * 64, kt * 128:(kt + 1) * 128],
                                                    AQ[comp * 64:(comp + 1) * 64, b * 128:(b + 1) * 128], start=True, stop=True)
                                return last
                            s.pe(fn, r=["aQ", "aK"], w=[f"ps{bi}"])
                            groups.append((kts, bi))
                        sbanks[i] = groups

                    def emit_rest(i):
                        tg, comp, bl = items[i]
                        b = tg * 4 + bl
                        pj = i % 2
                        for kts, bi in sbanks.pop(i):
                            n = len(kts) * 128
                            s.act(lambda e, kts=kts, bi=bi, n=n: e.activation(out=PTa[pj][:, kts[0] * 128:kts[0] * 128 + n],
                                                                             in_=c.psum[bi][:, 0:n], func=AF.Exp, scale=0.125),
                                  r=[f"ps{bi}"], w=[f"apt{pj}"])
                        s.dve(lambda e: e.memset(PTa[pj][64:128, b * 128:b * 128 + 64], 0.0), w=[f"apt{pj}"])
                        if i + 1 < len(items):
                            emit_S(i + 1)
                        ob = 4 + i % 2

                        def fn(e):
                            last = None
                            for kt in range(b + 1):
                                e.matmul(c.psum[ob][:, 0:128], AV[:, kt, :], PTa[pj][:, kt * 128:(kt + 1) * 128],
                                         start=(kt == 0), stop=(kt == b))
                            for kt in range(b + 1):
                                last = e.matmul(c.psum[ob][:, 128:256], c.onesb[:], PTa[pj][:, kt * 128:(kt + 1) * 128],
                                                start=(kt == 0), stop=(kt == b))
                            return last
                        s.pe(fn, r=[f"apt{pj}", "aV", "onesb"], w=[f"ps{ob}"])
                        s.act(lambda e: e.copy(out=Ob[comp][:, bl, :], in_=c.psum[ob][:, 0:256]), r=[f"ps{ob}"], w=[f"aob{comp}"])
                        if comp == 1 and bl == 3:
                            combine(tg)

                    def combine(tg):
                        O1, s1 = Ob[0][:, :, 0:128], Ob[0][:, :, 128:256]
                        O2, s2 = Ob[1][:, :, 0:128], Ob[1][:, :, 128:256]
                        s.dve(lambda e: e.reciprocal(out=tA[:], in_=s1), r=["aob0"], w=["tA"])
                        s.dve(lambda e: e.tensor_tensor(out=tA[:], in0=O1, in1=tA[:], op=ALU.mult), r=["aob0", "tA"], w=["tA"])
                        s.dve(lambda e: e.reciprocal(out=tB[:], in_=s2), r=["aob1"], w=["tB"])
                        s.dve(lambda e: e.tensor_tensor(out=tB[:], in0=O2, in1=tB[:], op=ALU.mult), r=["aob1", "tB"], w=["tB"])
                        s.dve(lambda e: e.scalar_tensor_tensor(out=tA[:], in0=tB[:], scalar=sc[:, 2:3], in1=tA[:], op0=ALU.mult, op1=ALU.add),
                              r=["tA", "tB", "sc"], w=["tA"])
                        tAf = tA[:].rearrange("p a b -> p (a b)")
                        tBf = tB[:].rearrange("p a b -> p (a b)")
                        s.act(lambda e: e.activation(out=tBf, in_=tAf, func=AF.Square), r=["tA"], w=["tB"])
                        s.pe(lambda e: e.matmul(c.psum[7][:], c.ones[:], tBf, start=True, stop=True), r=["tB", "ones"], w=["ps7"])
                        s.act(lambda e: e.activation(out=tBf, in_=c.psum[7][:], func=AF.Sqrt, scale=1.0 / 128, bias=c.epsb[:]),
                              r=["ps7", "epsb"], w=["tB"])
                        s.dve(lambda e: e.reciprocal(out=tBf, in_=tBf), r=["tB"], w=["tB"])
                        s.dve(lambda e: e.scalar_tensor_tensor(out=OT[:, h, tg * TG:(tg + 1) * TG], in0=tAf, scalar=sc[:, 3:4], in1=tBf,
                                                               op0=ALU.mult, op1=ALU.mult),
                              r=["tA", "tB", "sc"], w=["OT"])
                    emit_S(0)
                    for i in range(len(items)):
                        emit_rest(i)
                    s.flush()

            if EVEN_DBG == "A":
                s.dve(lambda e: e.memset(OT[:, 4:8, :], 0.0), w=["OT"])
                s.flush()
            for j in (range(4) if EVEN_DBG != "A" else []):
                Wt, wtok, _ = load_w(1536 + j * 128)
                rope_proj(Wt, wtok, lambda tg, j=j: BqT(j)[:, tg * TG:(tg + 1) * TG], "bQ", 0, 0)
                Wt, wtok, _ = load_w(2048 + j * 128)
                rope_proj(Wt, wtok, lambda tg, j=j: BkT(j)[:, tg * TG:(tg + 1) * TG], "bK", 0, 0)
            for j in (range(4) if EVEN_DBG != "A" else []):
                Wt, wtok, _ = load_w(2560 + j * 128)
                for t4 in range(4):
                    bank = c.psum[4 + t4 % 2]
                    bk = f"ps{4 + t4 % 2}"

                    def mmv(e, bank=bank, t4=t4, Wt=Wt):
                        last = None
                        for jj in range(4):
                            tt = t4 * 4 + jj
                            for dc in range(NDC):
                                last = e.matmul(bank[:, jj * 128:(jj + 1) * 128], hT[:, dc, tt * 128:(tt + 1) * 128], Wt[:, dc, :],
                                                start=(dc == 0), stop=(dc == NDC - 1))
                        return last
                    s.pe(mmv, r=[wtok, f"h:{t4}"], w=[bk])
                    for jj in range(4):
                        tt = t4 * 4 + jj
                        s.dve(lambda e, bank=bank, tt=tt, jj=jj, j=j: e.tensor_copy(out=Bv(tt)[:, j * 128:(j + 1) * 128],
                                                                                   in_=bank[:, jj * 128:(jj + 1) * 128]),
                              r=[bk], w=["bV"])
            for j in (range(2) if EVEN_DBG != "A" else []):
                Wt, wtok, _ = load_w(3072 + j * 128)
                rope_proj(Wt, wtok, lambda tg, j=j: qiT[:, j, tg * TG:(tg + 1) * TG], "qi", 2, 1)
            slot = wcnt[0] % 3
            wcnt[0] += 1
            for r4 in (range(4) if EVEN_DBG != "A" else []):
                s.dma("pool", W[slot][:, :, r4 * 32:(r4 + 1) * 32], w_in[:, :, 3328:3360], f"ewk{r4}",
                      r=([] if r4 == 0 else [f"ew{slot}"]), w=([f"ew{slot}", "ewk0"] if r4 == 0 else [f"ewk{r4}"]))
            if EVEN_DBG != "A":
                rope_proj(W[slot], [f"ew{slot}"] + [f"ewk{r4}" for r4 in range(4)], lambda tg: kiT4[:, tg * TG:(tg + 1) * TG], "ki", 2, 1)
            Wt, wtok, _ = load_w(3360, ncol=8)

            def mmw(e):
                last = None
                for tt in range(16):
                    for dc in range(NDC):
                        last = e.matmul(c.psum[5][:, tt * 8:(tt + 1) * 8], hT[:, dc, tt * 128:(tt + 1) * 128], Wt[:, dc, 0:8],
                                        start=(dc == 0), stop=(dc == NDC - 1))
                return last
            s.pe(mmw, r=[wtok] + [f"h:{tg}" for tg in range(4)], w=["ps5"])
            s.act(lambda e: e.activation(out=wq[:], in_=c.psum[5][:, 0:128], func=AF.Copy, scale=float(8 ** -0.5 * 32 ** -0.5)),
                  r=["ps5"], w=["wq"])
            s.flush()

        with ExitStack() as eb:
            def sbb(name, shape, dt):
                return eb.enter_context(_sbt(nc, name, shape, dt))
            nm = sbb("b_nm", [128, S], BF16)
            tb = [sbb(f"b_tb{j}", [128, TG], F32) for j in range(2)]
            m8 = sbb("b_m8", [128, 8], F32)
            PTb = [sbb(f"b_pt{j}", [128, S], BF16) for j in range(2)]
            recb = [sbb(f"b_rec{j}", [128, 128], F32) for j in range(2)]
            scnt = 0
            gcnt = 0
            if EVEN_DBG in ("A", "AB"):
                s.dve(lambda e: e.memset(OT[:, 4:8, :], 0.0), w=["OT"])
                s.flush()
            for t in (range(16) if EVEN_DBG not in ("A", "AB") else []):
                n = 128 * (t + 1)
                if t >= 2:
                    for h in range(8):
                        p0 = (h % 4) * 32
                        for kb in range((n + 511) // 512):
                            ncol = min(512, n - kb * 512)
                            bi = scnt % 2
                            scnt += 1
                            s.pe(lambda e, h=h, p0=p0, kb=kb, ncol=ncol, bi=bi: e.matmul(
                                c.psum[bi][:, 0:ncol], qiT[p0:p0 + 32, h // 4, t * 128:(t + 1) * 128],
                                kiT4[p0:p0 + 32, kb * 512:kb * 512 + ncol], start=True, stop=True, tile_position=(p0, 0)),
                                r=["qi", "ki"], w=[f"ps{bi}"])
                            if h == 0:
                                s.dve(lambda e, kb=kb, ncol=ncol, bi=bi, h=h: e.tensor_scalar(
                                    out=acc[:, kb * 512:kb * 512 + ncol], in0=c.psum[bi][:, 0:ncol], scalar1=0.0,
                                    scalar2=wq[:, t * 8 + h:t * 8 + h + 1], op0=ALU.max, op1=ALU.mult),
                                    r=[f"ps{bi}", "wq"], w=[f"acc{kb}"])
                            else:
                                s.dve(lambda e, kb=kb, ncol=ncol, bi=bi, h=h: e.tensor_scalar(
                                    out=tb[bi][:, 0:ncol], in0=c.psum[bi][:, 0:ncol], scalar1=0.0,
                                    scalar2=wq[:, t * 8 + h:t * 8 + h + 1], op0=ALU.max, op1=ALU.mult),
                                    r=[f"ps{bi}", "wq"], w=[f"tb{bi}"])
                                s.pool(lambda e, kb=kb, ncol=ncol, bi=bi: e.tensor_tensor(
                                    out=acc[:, kb * 512:kb * 512 + ncol], in0=acc[:, kb * 512:kb * 512 + ncol],
                                    in1=tb[bi][:, 0:ncol], op=ALU.add),
                                    r=[f"tb{bi}", f"acc{kb}"], w=[f"acc{kb}"])
                    acct = [f"acc{kb}" for kb in range(4)]
                    s.dve(lambda e: e.memset(acc[0:64, t * 128 + 64:t * 128 + 128], NEG), r=acct, w=acct)
                    for rnd in range(32):
                        src = acc if rnd == 0 else work
                        stok = acct if rnd == 0 else ["work"]
                        s.dve(lambda e, src=src: e.max(out=m8[:], in_=src[:, 0:n]), r=stok, w=["m8"])
                        if rnd < 31:
                            s.dve(lambda e, src=src: e.match_replace(out=work[:, 0:n], in_to_replace=m8[:], in_values=src[:, 0:n],
                                                                     imm_value=NEG),
                                  r=stok + ["m8"], w=["work"])
                    s.dve(lambda e: e.tensor_scalar(out=nm[:, 0:n], in0=acc[:, 0:n], scalar1=m8[:, 7:8], scalar2=-30000.0,
                                                    op0=ALU.is_lt, op1=ALU.mult),
                          r=acct + ["m8"], w=["nm"])
                else:
                    s.dve(lambda e: e.memset(nm[:, 0:n], 0.0), w=["nm"])
                    s.dve(lambda e: e.memset(nm[0:64, t * 128 + 64:t * 128 + 128], -30000.0), w=["nm"])
                for h in range(8):
                    r0 = (h % 2) * 64
                    pj = h % 2
                    glist = []
                    for g0 in range(0, t + 1, 4):
                        kts = list(range(g0, min(g0 + 4, t + 1)))
                        bi = 2 + gcnt % 3
                        gcnt += 1

                        def fn(e, kts=kts, bi=bi, h=h, r0=r0):
                            last = None
                            for j, kt in enumerate(kts):
                                e.matmul(c.psum[bi][:, j * 128:(j + 1) * 128], BkT(h // 2)[r0:r0 + 64, kt * 128:(kt + 1) * 128],
                                         BqT(h // 2)[r0:r0 + 64, t * 128:(t + 1) * 128], start=True, stop=False)
                                last = e.matmul(c.psum[bi][:, j * 128:(j + 1) * 128], nm[:, kt * 128:(kt + 1) * 128], c.identb[:],
                                                start=False, stop=True)
                            return last
                        s.pe(fn, r=["bQ", "bK", "nm", "identb"], w=[f"ps{bi}"])
                        nn = len(kts) * 128
                        s.act(lambda e, kts=kts, bi=bi, nn=nn, pj=pj: e.activation(out=PTb[pj][:, kts[0] * 128:kts[0] * 128 + nn],
                                                                                  in_=c.psum[bi][:, 0:nn], func=AF.Exp, scale=0.125),
                              r=[f"ps{bi}"], w=[f"bpt{pj}"])
                    ob = 6 + (h // 2) % 2

                    def fnpv(e, h=h, r0=r0, pj=pj, ob=ob):
                        last = None
                        for kt in range(t + 1):
                            e.matmul(c.psum[ob][r0:r0 + 64, 0:128], Bv(kt)[:, h * 64:(h + 1) * 64], PTb[pj][:, kt * 128:(kt + 1) * 128],
                                     start=(kt == 0), stop=(kt == t))
                        for kt in range(t + 1):
                            last = e.matmul(c.psum[ob][r0:r0 + 64, 128:256], c.onesb[:, 0:64], PTb[pj][:, kt * 128:(kt + 1) * 128],
                                            start=(kt == 0), stop=(kt == t))
                        return last
                    s.pe(fnpv, r=[f"bpt{pj}", "bV", "onesb"], w=[f"ps{ob}"])
                    if h % 2 == 1:
                        rj = (h // 2) % 2
                        s.dve(lambda e, ob=ob, rj=rj: e.reciprocal(out=recb[rj][:], in_=c.psum[ob][:, 128:256]), r=[f"ps{ob}"], w=[f"brec{rj}"])
                        s.dve(lambda e, ob=ob, rj=rj, h=h: e.tensor_tensor(out=OT[:, 4 + h // 2, t * 128:(t + 1) * 128], in0=c.psum[ob][:, 0:128],
                                                                          in1=recb[rj][:], op=ALU.mult),
                              r=[f"ps{ob}", f"brec{rj}"], w=["OT"])
                s.flush()
        s.dma("sp", c.xT[:].rearrange("p a b -> p (a b)"), c.xspill[:, :], "xsp2", r=["xspill"], w=[f"x:{tg}" for tg in range(4)])
        s.flush()
        out_proj(c, OT, c.e_w_out[0], gi3)


def host_consts(inputs):
    g = np.asarray(inputs["norm_g"], dtype=np.float32)
    g_lay = np.ascontiguousarray(g.reshape(12, 8, 128).transpose(2, 0, 1).reshape(128, 96))
    out = {"g_lay": g_lay, "ident": np.eye(128, dtype=np.float32)}
    rb = np.asarray(inputs["odd_rel_bias"], dtype=np.float32)[0]
    j = np.arange(128)[:, None]
    q = np.arange(640)[None, :]
    idx = np.clip(q - j, -63, 256) + 63
    out["odd_biasT"] = np.ascontiguousarray(rb[:, idx])
    cq, ck = q // 64, j // 64
    out["odd_mask"] = ((ck <= cq) & (cq <= ck + 8)).astype(np.float32)
    t = np.arange(S, dtype=np.float32)[None, :]
    tabs = np.zeros((4, 128, S), np.float32)
    for ti, dim in ((0, 64), (2, 32)):
        inv = (np.float32(10000.0) ** (-np.arange(0, dim, 2, dtype=np.float32) / np.float32(dim))).astype(np.float32)
        fi = (np.arange(128) % dim) % (dim // 2)
        ang = (t * inv[fi][:, None]).astype(np.float32)
        sign = np.where((np.arange(128) % dim) < dim // 2, 1.0, 1.0).astype(np.float32)
        tabs[ti] = np.cos(ang)
        tabs[ti + 1] = np.sin(ang) * sign[:, None]
    out["rope_tabs"] = tabs
    rm = np.zeros((2, 128, 128), np.float32)
    for ri, dim in ((0, 64), (1, 32)):
        for dst in range(128):
            jj = dst % dim
            if jj < dim // 2:
                rm[ri, dst + dim // 2, dst] = -1.0
            else:
                rm[ri, dst - dim // 2, dst] = 1.0
    out["rot_mats"] = rm
    lp = np.asarray(inputs["even_lambda"], dtype=np.float32)[0].reshape(1, 256)
    out["lam_lay"] = np.ascontiguousarray(np.broadcast_to(lp, (128, 256)))
    out["subln_lay"] = np.ascontiguousarray(np.asarray(inputs["even_subln"], dtype=np.float32)[0].reshape(128, 1))
    return out


N_LAUNCH = 1


def kernel(**inputs):
    x = np.ascontiguousarray(np.asarray(inputs["x"], dtype=np.float32))
    consts = host_consts(inputs)
    shared = dict(consts)
    for k in ("ffn_wg", "ffn_wu", "ffn_wd", "even_w_in", "even_w_out", "odd_w_in", "odd_w_out"):
        shared[k] = np.ascontiguousarray(np.asarray(inputs[k], dtype=np.float32))
    nseq = SEQ_PER_CORE // N_LAUNCH
    nc = build_program(nseq, stages=ALL_STAGES)
    out = np.empty_like(x)
    for li in range(N_LAUNCH):
        in_maps = []
        for ci in range(N_CORES):
            m = dict(shared)
            b0 = ci * SEQ_PER_CORE + li * nseq
            m["x"] = x[b0:b0 + nseq]
            in_maps.append(m)
        res = run_bass_kernel_spmd(nc, in_maps, core_ids=list(range(N_CORES)))
        for ci in range(N_CORES):
            b0 = ci * SEQ_PER_CORE + li * nseq
            out[b0:b0 + nseq] = res.results[ci]["y"]
    return out
```

```python
from contextlib import ExitStack
import numpy as np
import concourse.bass as bass
import concourse.mybir as mybir
from concourse.bass_utils import run_bass_kernel_spmd

F32 = mybir.dt.float32
BF16 = mybir.dt.bfloat16
AF = mybir.ActivationFunctionType
ALU = mybir.AluOpType

D = 1024
S = 2048
FF = 2816
NDC = 8
NFC = 22
TG = 512
EPS = 1e-6
N_CORES = 8
SEQ_PER_CORE = 2

ENGS = ("pe", "act", "dve", "pool", "sp")
BLOCKNAME = {"pe": "tensor", "act": "scalar", "dve": "vector", "pool": "gpsimd", "sp": "sync"}


class Op:
    __slots__ = ("eng", "fn", "deps", "is_dma", "semname", "val", "signal", "block")

    def __init__(self, eng, fn, block):
        self.eng = eng
        self.fn = fn
        self.deps = []
        self.is_dma = False
        self.semname = None
        self.val = 0
        self.signal = False
        self.block = block


class Sched:
    def __init__(self, nc, same_eng_sync=True):
        self.nc = nc
        self.same_eng_sync = same_eng_sync
        self.cur = {e: [] for e in ENGS}
        self.block_id = 0
        self.last_w = {}
        self.readers = {}
        self.sems = {}
        self.cnt = {e: 0 for e in ENGS}
        self.dma_cnt = {}
        self.waited = {e: {} for e in ENGS}
        self.stack = ExitStack()
        self.nops = 0

    def sem(self, name):
        if name not in self.sems:
            self.sems[name] = self.stack.enter_context(self.nc.semaphore(name))
        return self.sems[name]

    def add(self, eng, fn, r=(), w=(), dma_key=None):
        op = Op(eng, fn, self.block_id)
        self.nops += 1
        if dma_key is not None:
            op.is_dma = True
            op.semname = "d_" + dma_key
            self.dma_cnt[dma_key] = self.dma_cnt.get(dma_key, 0) + 1
            op.val = 16 * self.dma_cnt[dma_key]
            op.signal = True
            self.sem(op.semname)
        psr = [t for t in r if t.startswith("ps") and t[2:].isdigit()]
        if psr:
            w = list(w) + [t for t in psr if t not in w]
            r = [t for t in r if t not in psr]
        deps = []
        for t in r:
            lw = self.last_w.get(t)
            if lw is not None:
                deps.append(lw)
        for t in w:
            lw = self.last_w.get(t)
            if lw is not None:
                deps.append(lw)
            deps.extend(self.readers.get(t, ()))
        for t in r:
            self.readers.setdefault(t, []).append(op)
        for t in w:
            self.last_w[t] = op
            self.readers[t] = []
        seen = set()
        for d in deps:
            if d is op or id(d) in seen:
                continue
            seen.add(id(d))
            if not d.is_dma:
                if d.block < self.block_id:
                    continue
                if d.eng == eng and not op.is_dma and (not self.same_eng_sync or eng == "pe"):
                    continue
            d.signal = True
            op.deps.append(d)
        self.cur[eng].append(op)
        return op

    def pe(self, fn, r=(), w=()):
        return self.add("pe", fn, r, w)

    def act(self, fn, r=(), w=()):
        return self.add("act", fn, r, w)

    def dve(self, fn, r=(), w=()):
        return self.add("dve", fn, r, w)

    def pool(self, fn, r=(), w=()):
        return self.add("pool", fn, r, w)

    def dma(self, eng, out, in_, key, r=(), w=()):
        return self.add(eng, lambda e: e.dma_start(out=out, in_=in_), r, w, dma_key=key)

    def flush(self):
        cur = self.cur
        if not any(cur[e] for e in ENGS):
            return
        for e in ENGS:
            for op in cur[e]:
                if not op.is_dma and op.signal:
                    self.cnt[e] += 1
                    op.val = self.cnt[e]
                    op.semname = "c_" + e
                    self.sem(op.semname)
        with self.nc.Block() as block:
            for e in ENGS:
                if not cur[e]:
                    continue

                def body(engine, e=e):
                    wt = self.waited[e]
                    for op in cur[e]:
                        for d in sorted(op.deps, key=lambda d: -d.val):
                            if wt.get(d.semname, 0) >= d.val:
                                continue
                            engine.wait_ge(self.sems[d.semname], d.val)
                            wt[d.semname] = d.val
                        ins = op.fn(engine)
                        if op.signal:
                            ins.then_inc(self.sems[op.semname], 16 if op.is_dma else 1)

                getattr(block, BLOCKNAME[e])(body)
        self.cur = {e: [] for e in ENGS}
        self.block_id += 1

    def wait_all_dma(self, eng, ops):
        def fn(engine):
            last = None
            for o in ops:
                last = engine.wait_ge(self.sems[o.semname], o.val)
            return last
        self.cur[eng].append(_mk_plain(eng, fn, self.block_id))


def _mk_plain(eng, fn, block):
    op = Op(eng, fn, block)
    return op


class Ctx:
    pass


_SBT_CNT = [0]


def _sbt(nc, name, shape, dt):
    _SBT_CNT[0] += 1
    return nc.sbuf_tensor(f"{name}_u{_SBT_CNT[0]}", shape, dt)


import os
EVEN_DBG = os.environ.get("EVEN_DBG", "")
LAST_INPUT_NAMES = set()
ALL_STAGES = ("ffn00", "mix0", "ffn01", "ffn10", "mix1", "ffn11")


def build_program(nseq=SEQ_PER_CORE, stages=ALL_STAGES, same_eng_sync=True):
    nc = bass.Bass("TRN2", target_bir_lowering=False)
    c = Ctx()
    c.nc = nc
    c.x = nc.dram_tensor("x", [nseq, S, D], F32, kind="ExternalInput").ap()
    c.y = nc.dram_tensor("y", [nseq, S, D], F32, kind="ExternalOutput").ap()
    c.g_lay = nc.dram_tensor("g_lay", [128, 96], F32, kind="ExternalInput").ap()
    c.ident = nc.dram_tensor("ident", [128, 128], F32, kind="ExternalInput").ap()
    c.wg = nc.dram_tensor("ffn_wg", [2, 2, D, FF], F32, kind="ExternalInput").ap()
    c.wu = nc.dram_tensor("ffn_wu", [2, 2, D, FF], F32, kind="ExternalInput").ap()
    c.wd = nc.dram_tensor("ffn_wd", [2, 2, FF, D], F32, kind="ExternalInput").ap()

    c.e_w_in = nc.dram_tensor("even_w_in", [1, D, 3368], F32, kind="ExternalInput").ap()
    c.e_w_out = nc.dram_tensor("even_w_out", [1, D, D], F32, kind="ExternalInput").ap()
    c.o_w_in = nc.dram_tensor("odd_w_in", [1, D, 3 * D], F32, kind="ExternalInput").ap()
    c.o_w_out = nc.dram_tensor("odd_w_out", [1, D, D], F32, kind="ExternalInput").ap()
    c.biasT = nc.dram_tensor("odd_biasT", [16, 128, 640], F32, kind="ExternalInput").ap()
    c.bmask = nc.dram_tensor("odd_mask", [128, 640], F32, kind="ExternalInput").ap()
    c.rope = nc.dram_tensor("rope_tabs", [4, 128, S], F32, kind="ExternalInput").ap()
    c.rmat = nc.dram_tensor("rot_mats", [2, 128, 128], F32, kind="ExternalInput").ap()
    c.lam_lay = nc.dram_tensor("lam_lay", [128, 256], F32, kind="ExternalInput").ap()
    c.subln_lay = nc.dram_tensor("subln_lay", [128, 1], F32, kind="ExternalInput").ap()
    LAST_INPUT_NAMES.clear()
    LAST_INPUT_NAMES.update(["x", "g_lay", "ident", "ffn_wg", "ffn_wu", "ffn_wd", "even_w_in", "even_w_out",
                             "odd_w_in", "odd_w_out", "odd_biasT", "odd_mask", "rope_tabs", "rot_mats",
                             "lam_lay", "subln_lay"])
    sch = Sched(nc, same_eng_sync=same_eng_sync)
    c.s = sch
    es = ExitStack()
    with es, sch.stack:
        def sb(name, shape, dt):
            return es.enter_context(_sbt(nc, name, shape, dt))

        def ps(name, shape, dt):
            return es.enter_context(nc.psum_tensor(name, shape, dt))

        c.xT = sb("xT", [128, NDC, S], F32)
        c.g = sb("g", [128, 96], F32)
        c.gh = sb("gh", [128, 96], F32)
        c.identf = sb("identf", [128, 128], F32)
        c.ones = sb("ones", [128, 128], F32)
        c.epsb = sb("epsb", [128, 1], F32)
        c.identb = sb("identb", [128, 128], BF16)
        c.onesb = sb("onesb", [128, 128], BF16)
        c.P = ps("psall", [128, 8 * TG], F32)
        c.psum = [c.P[:, i * TG:(i + 1) * TG] for i in range(8)]

        sch.dma("sp", c.g[:], c.g_lay[:, :], "c0", w=["g"])
        sch.dma("sp", c.identf[:], c.ident[:, :], "c1", w=["ident"])
        sch.dve(lambda e: e.memset(c.ones[:], 1.0), w=["ones"])
        sch.dve(lambda e: e.memset(c.epsb[:], EPS), w=["epsb"])
        sch.dve(lambda e: e.memset(c.onesb[:], 1.0), w=["onesb"])
        sch.dve(lambda e: e.tensor_copy(out=c.identb[:], in_=c.identf[:]), r=["ident"], w=["identb"])
        sch.dve(lambda e: e.tensor_scalar_mul(c.gh[:], c.g[:], 0.5), r=["g"], w=["gh"])
        sch.flush()

        for sq in range(nseq):
            c.xspill = c.y[sq].rearrange("(p a) d -> p (a d)", p=128)
            load_x(c, sq)
            sch.flush()
            for st in stages:
                if st.startswith("ffn"):
                    l, i = int(st[3]), int(st[4])
                    ffn(c, l, i)
                elif st == "mix0":
                    mixer_even(c)
                elif st == "mix1":
                    mixer_odd(c)
                sch.flush()
            store_x(c, sq)
            sch.flush()
    return nc


def load_x(c, sq):
    nc, s = c.nc, c.s
    with ExitStack() as es:
        xin = [es.enter_context(_sbt(nc, f"xin{i}", [128, D], F32)) for i in range(2)]
        for tt in range(S // 128):
            b = tt % 2
            s.dma("sp", xin[b][:], c.x[sq, tt * 128:(tt + 1) * 128, :], f"xin{b}", w=[f"xin{b}"])
            for half in range(2):
                bank = c.psum[(2 * tt + half) % 4]
                bk = f"ps{(2 * tt + half) % 4}"

                def tr(e, bank=bank, b=b, half=half):
                    last = None
                    for j in range(4):
                        dc = half * 4 + j
                        last = e.transpose(bank[:, j * 128:(j + 1) * 128], xin[b][:, dc * 128:(dc + 1) * 128], c.identf[:])
                    return last
                s.pe(tr, r=[f"xin{b}", "ident"], w=[bk])
                dst = c.xT[:, half * 4:half * 4 + 4, tt * 128:(tt + 1) * 128]
                src = bank[:].rearrange("p (j t) -> p j t", j=4)
                if half == 0:
                    s.act(lambda e, dst=dst, src=src: e.copy(out=dst, in_=src), r=[bk], w=[f"xl:{tt}:a"])
                else:
                    s.dve(lambda e, dst=dst, src=src: e.tensor_copy(out=dst, in_=src), r=[bk], w=[f"xl:{tt}:b"])
        s.flush()


def store_x(c, sq):
    nc, s = c.nc, c.s
    with ExitStack() as es:
        xo = [es.enter_context(_sbt(nc, f"xo{i}", [128, D], F32)) for i in range(2)]
        outs = []
        for tt in range(S // 128):
            b = tt % 2
            for half in range(2):
                bank = c.psum[(2 * tt + half) % 4]
                bk = f"ps{(2 * tt + half) % 4}"

                def tr(e, bank=bank, half=half, tt=tt):
                    last = None
                    for j in range(4):
                        dc = half * 4 + j
                        last = e.transpose(bank[:, j * 128:(j + 1) * 128], c.xT[:, dc, tt * 128:(tt + 1) * 128], c.identf[:])
                    return last
                s.pe(tr, r=[f"x:{tt // 4}", "ident"], w=[bk])
                dst = xo[b][:, half * 512:(half + 1) * 512]
                if half == 0:
                    s.act(lambda e, dst=dst, bank=bank: e.copy(out=dst, in_=bank[:]), r=[bk], w=[f"xo{b}"])
                else:
                    s.dve(lambda e, dst=dst, bank=bank: e.tensor_copy(out=dst, in_=bank[:]), r=[bk], w=[f"xo{b}"])
            outs.append(s.dma("sp", c.y[sq, tt * 128:(tt + 1) * 128, :], xo[b][:], f"xo{b}", r=[f"xo{b}"]))
        s.wait_all_dma("sp", outs[-2:])
        s.flush()


def rms_rstd(c, ss_bank, bk, tmp, rstd, tok_rstd):
    s = c.s
    s.act(lambda e: e.activation(out=tmp[:], in_=ss_bank[:], func=AF.Sqrt, scale=1.0 / D, bias=c.epsb[:]),
          r=[bk, "epsb"], w=["rtmp"])
    s.dve(lambda e: e.reciprocal(out=rstd[:], in_=tmp[:]), r=["rtmp"], w=[tok_rstd])


def ffn(c, l, i):
    nc, s = c.nc, c.s
    gi0 = (l * 6 + (0 if i == 0 else 4)) * 8
    gi1 = (l * 6 + (1 if i == 0 else 5)) * 8
    wg = c.wg[l, i].rearrange("(c p) f -> p c f", p=128)
    wu = c.wu[l, i].rearrange("(c p) f -> p c f", p=128)
    wd = c.wd[l, i].rearrange("(k p) d -> p k d", p=128)
    NW = 3
    with ExitStack() as es:
        def sb(name, shape, dt):
            return es.enter_context(_sbt(nc, name, shape, dt))
        hT = sb("f_hT", [128, NDC, 1024], BF16)
        AT = sb("f_AT", [128, NFC, 1024], BF16)
        Yb = sb("f_Y", [128, NDC, 1024], F32)
        wgs = [sb(f"f_wg{j}", [128, NDC, 256], BF16) for j in range(NW)]
        wus = [sb(f"f_wu{j}", [128, NDC, 256], BF16) for j in range(NW)]
        wds = [sb(f"f_wd{j}", [128, NFC, 128], BF16) for j in range(2)]
        sq = [sb(f"f_sq{j}", [128, TG], F32) for j in range(2)]
        sg = [sb(f"f_sg{j}", [128, TG], F32) for j in range(2)]
        rtmp = sb("f_rtmp", [128, TG], F32)
        rstd = [sb(f"f_rstd{j}", [128, TG], F32) for j in range(2)]

        for hf in range(2):
            t0 = hf * 1024
            for tgl in range(2):
                tok = t0 + tgl * TG
                xg = f"x:{tok // TG}"
                bank = c.psum[6 + tgl]
                bk = f"ps{6 + tgl}"
                for dc in range(NDC):
                    j = dc % 2
                    s.act(lambda e, j=j, dc=dc, tok=tok: e.activation(out=sq[j][:], in_=c.xT[:, dc, tok:tok + TG], func=AF.Square),
                          r=[xg], w=[f"sq{j}"])
                    s.pe(lambda e, j=j, dc=dc, bank=bank: e.matmul(bank[:], c.ones[:], sq[j][:], start=(dc == 0), stop=(dc == NDC - 1)),
                         r=[f"sq{j}", "ones"], w=[bk])
                rms_rstd(c, bank, bk, rtmp, rstd[tgl], f"rstd{tgl}")
                for dc in range(NDC):
                    s.dve(lambda e, dc=dc, tok=tok, tgl=tgl: e.scalar_tensor_tensor(
                        out=hT[:, dc, tgl * TG:(tgl + 1) * TG], in0=c.xT[:, dc, tok:tok + TG],
                        scalar=c.g[:, gi0 + dc:gi0 + dc + 1], in1=rstd[tgl][:], op0=ALU.mult, op1=ALU.mult),
                        r=[xg, f"rstd{tgl}", "g"], w=[f"h:{tgl}"])
            k = 0
            for gq in range(NFC // 2):
                slot = gq % NW
                s.dma("pool", wgs[slot][:], wg[:, :, gq * 256:(gq + 1) * 256], f"wg{slot}", w=[f"wg{slot}"])
                s.dma("pool", wus[slot][:], wu[:, :, gq * 256:(gq + 1) * 256], f"wu{slot}", w=[f"wu{slot}"])
                for j in range(2):
                    ffc = 2 * gq + j
                    for tgl in range(2):
                        bG, bU = c.psum[2 * (k % 2)], c.psum[2 * (k % 2) + 1]
                        kG, kU = f"ps{2 * (k % 2)}", f"ps{2 * (k % 2) + 1}"
                        sgk = sg[k % 2]

                        def mmg(e, W, bank, j=j, tgl=tgl):
                            last = None
                            for dc in range(NDC):
                                last = e.matmul(bank[:], W[:, dc, j * 128:(j + 1) * 128], hT[:, dc, tgl * TG:(tgl + 1) * TG],
                                                start=(dc == 0), stop=(dc == NDC - 1))
                            return last
                        s.pe(lambda e, W=wgs[slot], bank=bG, f=mmg: f(e, W, bank), r=[f"wg{slot}", f"h:{tgl}"], w=[kG])
                        s.pe(lambda e, W=wus[slot], bank=bU, f=mmg: f(e, W, bank), r=[f"wu{slot}", f"h:{tgl}"], w=[kU])
                        s.act(lambda e, sgk=sgk, bG=bG: e.activation(out=sgk[:], in_=bG[:], func=AF.Silu), r=[kG], w=[f"sg{k % 2}"])
                        s.dve(lambda e, sgk=sgk, bU=bU, ffc=ffc, tgl=tgl: e.tensor_tensor(
                            out=AT[:, ffc, tgl * TG:(tgl + 1) * TG], in0=sgk[:], in1=bU[:], op=ALU.mult),
                            r=[kU, f"sg{k % 2}"], w=[f"AT:{tgl}"])
                        k += 1
            pend = None
            k = 0
            for dc in range(NDC):
                slot = dc % 2
                s.dma("pool", wds[slot][:], wd[:, :, dc * 128:(dc + 1) * 128], f"wd{slot}", w=[f"wd{slot}"])
                for tgl in range(2):
                    bY = c.psum[4 + (k % 2)]
                    kY = f"ps{4 + (k % 2)}"

                    def mmy(e, W=wds[slot], bank=bY, tgl=tgl):
                        last = None
                        for kc in range(NFC):
                            last = e.matmul(bank[:], W[:, kc, :], AT[:, kc, tgl * TG:(tgl + 1) * TG],
                                            start=(kc == 0), stop=(kc == NFC - 1))
                        return last
                    s.pe(mmy, r=[f"wd{slot}", f"AT:{tgl}"], w=[kY])
                    if pend is not None:
                        pend()
                    sqk = sq[k % 2]
                    s.act(lambda e, bY=bY, dc=dc, tgl=tgl: e.copy(out=Yb[:, dc, tgl * TG:(tgl + 1) * TG], in_=bY[:]),
                          r=[kY], w=[f"Y:{tgl}"])
                    s.act(lambda e, bY=bY, sqk=sqk: e.activation(out=sqk[:], in_=bY[:], func=AF.Square),
                          r=[kY], w=[f"sq{k % 2}"])

                    def ssmm(sqk=sqk, k=k, dc=dc, tgl=tgl):
                        bank = c.psum[6 + tgl]
                        s.pe(lambda e: e.matmul(bank[:], c.ones[:], sqk[:], start=(dc == 0), stop=(dc == NDC - 1)),
                             r=[f"sq{k % 2}", "ones"], w=[f"ps{6 + tgl}"])
                    pend = ssmm
                    k += 1
            pend()
            for tgl in range(2):
                tok = t0 + tgl * TG
                xg = f"x:{tok // TG}"
                rms_rstd(c, c.psum[6 + tgl], f"ps{6 + tgl}", rtmp, rstd[tgl], f"rstd{tgl}")
                for dc in range(NDC):
                    ysl = Yb[:, dc, tgl * TG:(tgl + 1) * TG]
                    s.dve(lambda e, ysl=ysl, tgl=tgl: e.tensor_tensor(out=ysl, in0=ysl, in1=rstd[tgl][:], op=ALU.mult),
                          r=[f"Y:{tgl}", f"rstd{tgl}"], w=[f"Y:{tgl}"])
                    xs = c.xT[:, dc, tok:tok + TG]
                    s.dve(lambda e, ysl=ysl, xs=xs, dc=dc: e.scalar_tensor_tensor(
                        out=xs, in0=ysl, scalar=c.gh[:, gi1 + dc:gi1 + dc + 1], in1=xs, op0=ALU.mult, op1=ALU.add),
                        r=[f"Y:{tgl}", "gh", xg], w=[xg])
        s.flush()


def rms_hT(c, hT, gi):
    nc, s = c.nc, c.s
    with ExitStack() as es:
        sq = [es.enter_context(_sbt(nc, f"r_sq{j}", [128, TG], F32)) for j in range(2)]
        rtmp = es.enter_context(_sbt(nc, "r_rtmp", [128, TG], F32))
        rstd = [es.enter_context(_sbt(nc, f"r_rstd{j}", [128, TG], F32)) for j in range(2)]
        for tg in range(4):
            tok = tg * TG
            xg = f"x:{tg}"
            bank = c.psum[6 + tg % 2]
            bk = f"ps{6 + tg % 2}"
            for dc in range(NDC):
                j = dc % 2
                s.act(lambda e, j=j, dc=dc, tok=tok: e.activation(out=sq[j][:], in_=c.xT[:, dc, tok:tok + TG], func=AF.Square),
                      r=[xg], w=[f"sq{j}"])
                s.pe(lambda e, j=j, dc=dc, bank=bank: e.matmul(bank[:], c.ones[:], sq[j][:], start=(dc == 0), stop=(dc == NDC - 1)),
                     r=[f"sq{j}", "ones"], w=[bk])
            rms_rstd(c, bank, bk, rtmp, rstd[tg % 2], f"rstd{tg % 2}")
            for dc in range(NDC):
                s.dve(lambda e, dc=dc, tok=tok, tg=tg: e.scalar_tensor_tensor(
                    out=hT[:, dc, tok:tok + TG], in0=c.xT[:, dc, tok:tok + TG],
                    scalar=c.g[:, gi + dc:gi + dc + 1], in1=rstd[tg % 2][:], op0=ALU.mult, op1=ALU.mult),
                    r=[xg, f"rstd{tg % 2}", "g"], w=[f"h:{tg}"])
        s.flush()


def out_proj(c, OT, W, gi):
    nc, s = c.nc, c.s
    Wv = W.rearrange("(k p) d -> p k d", p=128)
    with ExitStack() as es:
        def sb(name, shape, dt):
            return es.enter_context(_sbt(nc, name, shape, dt))
        Wb = sb("p_W", [128, NDC, D], BF16)
        Yb = sb("p_Y", [128, NDC, 1024], F32)
        sq = [sb(f"p_sq{j}", [128, TG], F32) for j in range(2)]
        rtmp = sb("p_rtmp", [128, TG], F32)
        rstd = [sb(f"p_rstd{j}", [128, TG], F32) for j in range(2)]
        s.dma("pool", Wb[:, 0:4, :], Wv[:, 0:4, :], "pw0", w=["pW0"])
        s.dma("pool", Wb[:, 4:8, :], Wv[:, 4:8, :], "pw1", w=["pW1"])
        for hf in range(2):
            pend = None
            k = 0
            for dc in range(NDC):
                for tgl in range(2):
                    tg = hf * 2 + tgl
                    bY = c.psum[4 + (k % 2)]
                    kY = f"ps{4 + (k % 2)}"

                    def mmy(e, bank=bY, tg=tg, dc=dc):
                        last = None
                        for kc in range(NDC):
                            last = e.matmul(bank[:], Wb[:, kc, dc * 128:(dc + 1) * 128], OT[:, kc, tg * TG:(tg + 1) * TG],
                                            start=(kc == 0), stop=(kc == NDC - 1))
                        return last
                    s.pe(mmy, r=["pW0", "pW1", "OT"], w=[kY])
                    if pend is not None:
                        pend()
                    sqk = sq[k % 2]
                    s.act(lambda e, bY=bY, dc=dc, tgl=tgl: e.copy(out=Yb[:, dc, tgl * TG:(tgl + 1) * TG], in_=bY[:]),
                          r=[kY], w=[f"Y:{tgl}"])
                    s.act(lambda e, bY=bY, sqk=sqk: e.activation(out=sqk[:], in_=bY[:], func=AF.Square),
                          r=[kY], w=[f"sq{k % 2}"])

                    def ssmm(sqk=sqk, k=k, dc=dc, tgl=tgl):
                        bank = c.psum[6 + tgl]
                        s.pe(lambda e: e.matmul(bank[:], c.ones[:], sqk[:], start=(dc == 0), stop=(dc == NDC - 1)),
                             r=[f"sq{k % 2}", "ones"], w=[f"ps{6 + tgl}"])
                    pend = ssmm
                    k += 1
            pend()
            for tgl in range(2):
                tg = hf * 2 + tgl
                tok = tg * TG
                xg = f"x:{tg}"
                rms_rstd(c, c.psum[6 + tgl], f"ps{6 + tgl}", rtmp, rstd[tgl], f"rstd{tgl}")
                for dc in range(NDC):
                    ysl = Yb[:, dc, tgl * TG:(tgl + 1) * TG]
                    s.dve(lambda e, ysl=ysl, tgl=tgl: e.tensor_tensor(out=ysl, in0=ysl, in1=rstd[tgl][:], op=ALU.mult),
                          r=[f"Y:{tgl}", f"rstd{tgl}"], w=[f"Y:{tgl}"])
                    xs = c.xT[:, dc, tok:tok + TG]
                    s.dve(lambda e, ysl=ysl, xs=xs, dc=dc: e.scalar_tensor_tensor(
                        out=xs, in0=ysl, scalar=c.g[:, gi + dc:gi + dc + 1], in1=xs, op0=ALU.mult, op1=ALU.add),
                        r=[f"Y:{tgl}", "g", xg], w=[xg])
        s.flush()


def mixer_odd(c):
    nc, s = c.nc, c.s
    gi2, gi3 = (6 + 2) * 8, (6 + 3) * 8
    w_in = c.o_w_in[0].rearrange("(c p) f -> p c f", p=128)
    with ExitStack() as eo:
        OT = eo.enter_context(_sbt(nc, "o_OT", [128, NDC, S], BF16))
        with ExitStack() as es:
            def sb(name, shape, dt):
                return es.enter_context(_sbt(nc, name, shape, dt))
            hT = sb("o_hT", [128, NDC, S], BF16)
            EB = sb("o_EB", [128, 16, 640], BF16)
            rms_hT(c, hT, gi2)
            with ExitStack() as e2:
                bt = [e2.enter_context(_sbt(nc, f"o_bt{j}", [128, 640], F32)) for j in range(2)]
                mk = e2.enter_context(_sbt(nc, "o_mk", [128, 640], F32))
                s.dma("sp", mk[:], c.bmask[:, :], "omk", w=["omk"])
                for h in range(16):
                    j = h % 2
                    s.dma("sp", bt[j][:], c.biasT[h], f"obt{j}", w=[f"obt{j}"])
                    s.act(lambda e, j=j: e.activation(out=bt[j][:], in_=bt[j][:], func=AF.Exp), r=[f"obt{j}"], w=[f"obt{j}"])
                    s.dve(lambda e, j=j, h=h: e.tensor_tensor(out=EB[:, h, :], in0=bt[j][:], in1=mk[:], op=ALU.mult),
                          r=[f"obt{j}", "omk"], w=["EB"])
                s.flush()
            Wq = [sb(f"o_wq{j}", [128, NDC, 128], BF16) for j in range(2)]
            Wk = [sb(f"o_wk{j}", [128, NDC, 128], BF16) for j in range(2)]
            Wv = [sb(f"o_wv{j}", [128, NDC, 128], BF16) for j in range(2)]
            QT = [sb(f"o_QT{j}", [128, S], BF16) for j in range(2)]
            KT = [sb(f"o_KT{j}", [128, S], BF16) for j in range(2)]
            V = [sb(f"o_V{j}", [128, 16, 128], BF16) for j in range(2)]
            PR = [sb(f"o_pr{j}", [128, 640], BF16) for j in range(2)]
            PT = [sb(f"o_pt{j}", [128, 640], BF16) for j in range(2)]
            rec = [sb(f"o_rec{j}", [128, 128], F32) for j in range(2)]
            pcl = [0]

            def hp_body(hp):
                    sl = hp % 2
                    s.dma("pool", Wq[sl][:], w_in[:, :, hp * 128:(hp + 1) * 128], f"owq{sl}", w=[f"owq{sl}"])
                    s.dma("pool", Wk[sl][:], w_in[:, :, D + hp * 128:D + (hp + 1) * 128], f"owk{sl}", w=[f"owk{sl}"])
                    s.dma("pool", Wv[sl][:], w_in[:, :, 2 * D + hp * 128:2 * D + (hp + 1) * 128], f"owv{sl}", w=[f"owv{sl}"])
                    for (W, dst, wtok, dtok) in ((Wq[sl], QT[sl], f"owq{sl}", f"oQ{sl}"), (Wk[sl], KT[sl], f"owk{sl}", f"oK{sl}")):
                        for tg in range(4):
                            bank = c.psum[pcl[0] % 2]
                            bk = f"ps{pcl[0] % 2}"
                            pcl[0] += 1

                            def mm(e, W=W, bank=bank, tg=tg):
                                last = None
                                for dc in range(NDC):
                                    last = e.matmul(bank[:], W[:, dc, :], hT[:, dc, tg * TG:(tg + 1) * TG],
                                                    start=(dc == 0), stop=(dc == NDC - 1))
                                return last
                            s.pe(mm, r=[wtok, f"h:{tg}"], w=[bk])
                            s.act(lambda e, dst=dst, bank=bank, tg=tg: e.copy(out=dst[:, tg * TG:(tg + 1) * TG], in_=bank[:]),
                                  r=[bk], w=[dtok])
                    for t4 in range(4):
                        bank = c.psum[2 + t4 % 2]
                        bk = f"ps{2 + t4 % 2}"

                        def mmv(e, bank=bank, t4=t4, W=Wv[sl]):
                            last = None
                            for j in range(4):
                                tt = t4 * 4 + j
                                for dc in range(NDC):
                                    last = e.matmul(bank[:, j * 128:(j + 1) * 128], hT[:, dc, tt * 128:(tt + 1) * 128], W[:, dc, :],
                                                    start=(dc == 0), stop=(dc == NDC - 1))
                            return last
                        s.pe(mmv, r=[f"owv{sl}", f"h:{t4}"], w=[bk])
                        s.dve(lambda e, bank=bank, t4=t4: e.tensor_copy(out=V[sl][:, t4 * 4:(t4 + 1) * 4, :],
                                                                       in_=bank[:].rearrange("p (a b) -> p a b", a=4)),
                              r=[bk], w=[f"oV{sl}"])
                    items = [(b, hh) for b in range(16) for hh in range(2)]

                    def kts_of(b):
                        return [kt for kt in range(b - 4, b + 1) if kt >= 0]

                    def emit_S(i):
                        b, hh = items[i]
                        sb0 = 2 * (i % 2)

                        def fn(e):
                            last = None
                            for kt in kts_of(b):
                                col = sb0 * TG + (b - kt) * 128
                                last = e.matmul(c.P[:, col:col + 128], KT[sl][hh * 64:(hh + 1) * 64, kt * 128:(kt + 1) * 128],
                                                QT[sl][hh * 64:(hh + 1) * 64, b * 128:(b + 1) * 128], start=True, stop=True)
                            return last
                        s.pe(fn, r=[f"oQ{sl}", f"oK{sl}"], w=[f"ps{sb0}", f"ps{sb0 + 1}"])

                    def emit_rest(i):
                        b, hh = items[i]
                        h = 2 * hp + hh
                        sb0 = 2 * (i % 2)
                        kts = kts_of(b)
                        ncols = len(kts) * 128
                        j = i % 2
                        s.act(lambda e: e.activation(out=PR[j][:, 0:ncols], in_=c.P[:, sb0 * TG:sb0 * TG + ncols], func=AF.Exp, scale=0.125),
                              r=[f"ps{sb0}", f"ps{sb0 + 1}"], w=[f"opr{j}"])
                        s.dve(lambda e: e.tensor_tensor(out=PT[j][:, 0:ncols], in0=PR[j][:, 0:ncols], in1=EB[:, h, 0:ncols], op=ALU.mult),
                              r=[f"opr{j}", "EB"], w=[f"opt{j}"])
                        if i + 1 < len(items):
                            emit_S(i + 1)
                        ob = 4 + 2 * (b % 2)

                        def fn(e):
                            last = None
                            n = len(kts)
                            for idx, kt in enumerate(kts):
                                r_ = b - kt
                                e.matmul(c.psum[ob][hh * 64:(hh + 1) * 64, 0:128], V[sl][:, kt, hh * 64:(hh + 1) * 64],
                                         PT[j][:, r_ * 128:(r_ + 1) * 128], start=(idx == 0), stop=(idx == n - 1))
                            for idx, kt in enumerate(kts):
                                r_ = b - kt
                                last = e.matmul(c.psum[ob + 1][hh * 64:(hh + 1) * 64, 0:128], c.onesb[:, 0:64],
                                                PT[j][:, r_ * 128:(r_ + 1) * 128], start=(idx == 0), stop=(idx == n - 1))
                            return last
                        s.pe(fn, r=[f"opt{j}", f"oV{sl}", "onesb"], w=[f"ps{ob}", f"ps{ob + 1}"])
                        if hh == 1:
                            rj = b % 2
                            s.dve(lambda e: e.reciprocal(out=rec[rj][:], in_=c.psum[ob + 1][:, 0:128]), r=[f"ps{ob + 1}"], w=[f"orec{rj}"])
                            s.dve(lambda e: e.tensor_tensor(out=OT[:, hp, b * 128:(b + 1) * 128], in0=c.psum[ob][:, 0:128],
                                                            in1=rec[rj][:], op=ALU.mult),
                                  r=[f"ps{ob}", f"orec{rj}"], w=["OT"])
                    emit_S(0)
                    for i in range(len(items)):
                        emit_rest(i)
            for hp in range(8):
                hp_body(hp)
            s.flush()
        out_proj(c, OT, c.o_w_out[0], gi3)


def mixer_even(c):
    nc, s = c.nc, c.s
    gi2, gi3 = 2 * 8, 3 * 8
    LAM_INIT = 0.8 - 0.6 * 1.0
    w_in = c.e_w_in[0].rearrange("(c p) f -> p c f", p=128)
    NEG = -1.0e30
    with ExitStack() as eo:
        def sbo(name, shape, dt):
            return eo.enter_context(_sbt(nc, name, shape, dt))
        OT = sbo("e_OT", [128, NDC, S], BF16)
        qiT = sbo("e_qiT", [128, 2, S], BF16)
        kiT4 = sbo("e_kiT", [128, S], BF16)
        wq = sbo("e_wq", [128, 128], F32)
        Bqv = c.xT[:, 0:2, :].bitcast(BF16)
        Bkv = c.xT[:, 2:4, :].bitcast(BF16)
        Bvv = c.xT[:, 4:6, :].bitcast(BF16)

        def BqT(j):
            return Bqv[:, j // 2, (j % 2) * S:(j % 2 + 1) * S]

        def BkT(j):
            return Bkv[:, j // 2, (j % 2) * S:(j % 2 + 1) * S]

        def Bv(kt):
            return Bvv[:, kt // 8, (kt % 8) * 512:(kt % 8 + 1) * 512]
        acc = c.xT[:, 6, :]
        work = c.xT[:, 7, :]
        with ExitStack() as es:
            def sb(name, shape, dt):
                return es.enter_context(_sbt(nc, name, shape, dt))
            hT = sb("e_hT", [128, NDC, S], BF16)
            rms_hT(c, hT, gi2)
            spill = s.dma("sp", c.xspill[:, :], c.xT[:].rearrange("p a b -> p (a b)"), "xsp", r=[f"x:{tg}" for tg in range(4)], w=["xspill"])
            s.wait_all_dma("sp", [spill])
            s.flush()
            tabs = [sb(f"e_tab{j}", [128, S], BF16) for j in range(4)]
            Rm = [sb(f"e_rm{j}", [128, 128], BF16) for j in range(2)]
            rmf = sb("e_rmf", [128, 128], F32)
            lam = sb("e_lam", [128, 256], F32)
            sc = sb("e_sc", [128, 8], F32)
            W = [sb(f"e_w{j}", [128, NDC, 128], BF16) for j in range(3)]
            Xb = sb("e_xb", [128, TG], BF16)
            t1 = sb("e_t1", [128, TG], F32)
            t2 = sb("e_t2", [128, TG], F32)
            for j in range(4):
                s.dma("pool", tabs[j][:], c.rope[j], f"etab{j}", w=[f"tab{j}"])
            for j in range(2):
                s.dma("sp", rmf[:], c.rmat[j], "ermf", w=["rmf"])
                s.dve(lambda e, j=j: e.tensor_copy(out=Rm[j][:], in_=rmf[:]), r=["rmf"], w=[f"rm{j}"])
            s.dma("sp", lam[:], c.lam_lay[:, :], "elam", w=["lam"])
            s.dma("sp", sc[:, 4:5], c.subln_lay[:, :], "esub", w=["sc4"])
            s.dve(lambda e: e.tensor_tensor(out=lam[:, 0:64], in0=lam[:, 0:64], in1=lam[:, 64:128], op=ALU.mult), r=["lam"], w=["lam"])
            s.dve(lambda e: e.tensor_tensor(out=lam[:, 128:192], in0=lam[:, 128:192], in1=lam[:, 192:256], op=ALU.mult), r=["lam"], w=["lam"])
            s.dve(lambda e: e.reduce_sum(out=sc[:, 0:1], in_=lam[:, 0:64], axis=mybir.AxisListType.X), r=["lam"], w=["sc"])
            s.dve(lambda e: e.reduce_sum(out=sc[:, 1:2], in_=lam[:, 128:192], axis=mybir.AxisListType.X), r=["lam"], w=["sc"])
            s.act(lambda e: e.activation(out=sc[:, 0:2], in_=sc[:, 0:2], func=AF.Exp), r=["sc"], w=["sc"])
            s.dve(lambda e: e.tensor_tensor(out=sc[:, 2:3], in0=sc[:, 1:2], in1=sc[:, 0:1], op=ALU.subtract), r=["sc"], w=["sc"])
            s.dve(lambda e: e.tensor_scalar_add(sc[:, 2:3], sc[:, 2:3], -LAM_INIT), r=["sc"], w=["sc"])
            s.dve(lambda e: e.tensor_scalar_mul(sc[:, 3:4], sc[:, 4:5], 1.0 - LAM_INIT), r=["sc", "sc4"], w=["sc"])
            s.flush()

            wcnt = [0]

            def load_w(col0, ncol=128, dst_col=0, slot=None):
                if slot is None:
                    slot = wcnt[0] % 3
                    wcnt[0] += 1
                s.dma("pool", W[slot][:, :, dst_col:dst_col + ncol], w_in[:, :, col0:col0 + ncol], f"ew{slot}", w=[f"ew{slot}"])
                return W[slot], f"ew{slot}", slot

            def rope_proj(Wt, wtok, dst_fn, dtok, ti, rmi):
                for tg in range(4):
                    b6, b7 = c.psum[6], c.psum[7]

                    def mm(e, tg=tg):
                        last = None
                        for dc in range(NDC):
                            last = e.matmul(b6[:], Wt[:, dc, :], hT[:, dc, tg * TG:(tg + 1) * TG], start=(dc == 0), stop=(dc == NDC - 1))
                        return last
                    s.pe(mm, r=(list(wtok) if isinstance(wtok, (list, tuple)) else [wtok]) + [f"h:{tg}"], w=["ps6"])
                    s.act(lambda e: e.copy(out=Xb[:], in_=b6[:]), r=["ps6"], w=["xb"])
                    s.pe(lambda e: e.matmul(b7[:], Rm[rmi][:], Xb[:], start=True, stop=True), r=["xb", f"rm{rmi}"], w=["ps7"])
                    s.dve(lambda e, tg=tg: e.tensor_tensor(out=t1[:], in0=b6[:], in1=tabs[ti][:, tg * TG:(tg + 1) * TG], op=ALU.mult),
                          r=["ps6", f"tab{ti}"], w=["t1"])
                    s.dve(lambda e, tg=tg: e.tensor_tensor(out=t2[:], in0=b7[:], in1=tabs[ti + 1][:, tg * TG:(tg + 1) * TG], op=ALU.mult),
                          r=["ps7", f"tab{ti + 1}"], w=["t2"])
                    s.pool(lambda e, tg=tg: e.tensor_tensor(out=dst_fn(tg), in0=t1[:], in1=t2[:], op=ALU.add),
                           r=["t1", "t2"], w=[dtok])

            with ExitStack() as ea:
                def sba(name, shape, dt):
                    return ea.enter_context(_sbt(nc, name, shape, dt))
                AQ = sba("a_Q", [128, S], BF16)
                AK = sba("a_K", [128, S], BF16)
                AV = sba("a_V", [128, 16, 128], BF16)
                PTa = [sba(f"a_pt{j}", [128, S], BF16) for j in range(2)]
                Ob = [sba(f"a_ob{j}", [128, 4, 256], F32) for j in range(2)]
                tA = sba("a_tA", [128, 4, 128], F32)
                tB = sba("a_tB", [128, 4, 128], F32)
                def a_head(h):
                        Wt, wtok, _ = load_w(h * 128)
                        rope_proj(Wt, wtok, lambda tg: AQ[:, tg * TG:(tg + 1) * TG], "aQ", 0, 0)
                        Wt, wtok, _ = load_w(512 + h * 128)
                        rope_proj(Wt, wtok, lambda tg: AK[:, tg * TG:(tg + 1) * TG], "aK", 0, 0)
                        Wt, wtok, _ = load_w(1024 + h * 128)
                        for t4 in range(4):
                            bank = c.psum[4 + t4 % 2]
                            bk = f"ps{4 + t4 % 2}"

                            def mmv(e, bank=bank, t4=t4, Wt=Wt):
                                last = None
                                for j in range(4):
                                    tt = t4 * 4 + j
                                    for dc in range(NDC):
                                        last = e.matmul(bank[:, j * 128:(j + 1) * 128], hT[:, dc, tt * 128:(tt + 1) * 128], Wt[:, dc, :],
                                                        start=(dc == 0), stop=(dc == NDC - 1))
                                return last
                            s.pe(mmv, r=[wtok, f"h:{t4}"], w=[bk])
                            s.dve(lambda e, bank=bank, t4=t4: e.tensor_copy(out=AV[:, t4 * 4:(t4 + 1) * 4, :],
                                                                           in_=bank[:].rearrange("p (a b) -> p a b", a=4)),
                                  r=[bk], w=["aV"])
                        items = [(tg, comp, bl) for tg in range(4) for comp in range(2) for bl in range(4)]
                        gcount = [0]
                        sbanks = {}

                        def emit_S(i):
                            tg, comp, bl = items[i]
                            b = tg * 4 + bl
                            groups = []
                            for g0 in range(0, b + 1, 4):
                                kts = list(range(g0, min(g0 + 4, b + 1)))
                                bi = gcount[0] % 4
                                gcount[0] += 1

                                def fn(e, kts=kts, bi=bi):
                                    last = None
                                    for j, kt in enumerate(kts):
                                        last = e.matmul(c.psum[bi][:, j * 128:(j + 1) * 128],
                                                        AK[comp * 64:(comp + 1) * 64, kt * 128:(kt + 1) * 128],
                                                        AQ[comp * 64:(comp + 1) * 64, b * 128:(b + 1) * 128], start=True, stop=True)
                                    return last
                                s.pe(fn, r=["aQ", "aK"], w=[f"ps{bi}"])
                                groups.append((kts, bi))
                            sbanks[i] = groups

                        def emit_rest(i):
                            tg, comp, bl = items[i]
                            b = tg * 4 + bl
                            pj = i % 2
                            for kts, bi in sbanks.pop(i):
                                n = len(kts) * 128
                                s.act(lambda e, kts=kts, bi=bi, n=n: e.activation(out=PTa[pj][:, kts[0] * 128:kts[0] * 128 + n],
                                                                                 in_=c.psum[bi][:, 0:n], func=AF.Exp, scale=0.125),
                                      r=[f"ps{bi}"], w=[f"apt{pj}"])
                            s.dve(lambda e: e.memset(PTa[pj][64:128, b * 128:b * 128 + 64], 0.0), w=[f"apt{pj}"])
                            if i + 1 < len(items):
                                emit_S(i + 1)
                            ob = 4 + i % 2

                            def fn(e):
                                last = None
                                for kt in range(b + 1):
                                    e.matmul(c.psum[ob][:, 0:128], AV[:, kt, :], PTa[pj][:, kt * 128:(kt + 1) * 128],
                                             start=(kt == 0), stop=(kt == b))
                                for kt in range(b + 1):
                                    last = e.matmul(c.psum[ob][:, 128:256], c.onesb[:], PTa[pj][:, kt * 128:(kt + 1) * 128],
                                                    start=(kt == 0), stop=(kt == b))
                                return last
                            s.pe(fn, r=[f"apt{pj}", "aV", "onesb"], w=[f"ps{ob}"])
                            s.act(lambda e: e.copy(out=Ob[comp][:, bl, :], in_=c.psum[ob][:, 0:256]), r=[f"ps{ob}"], w=[f"aob{comp}"])
                            if comp == 1 and bl == 3:
                                combine(tg)

                        def combine(tg):
                            O1, s1 = Ob[0][:, :, 0:128], Ob[0][:, :, 128:256]
                            O2, s2 = Ob[1][:, :, 0:128], Ob[1][:, :, 128:256]
                            s.dve(lambda e: e.reciprocal(out=tA[:], in_=s1), r=["aob0"], w=["tA"])
                            s.dve(lambda e: e.tensor_tensor(out=tA[:], in0=O1, in1=tA[:], op=ALU.mult), r=["aob0", "tA"], w=["tA"])
                            s.dve(lambda e: e.reciprocal(out=tB[:], in_=s2), r=["aob1"], w=["tB"])
                            s.dve(lambda e: e.tensor_tensor(out=tB[:], in0=O2, in1=tB[:], op=ALU.mult), r=["aob1", "tB"], w=["tB"])
                            s.dve(lambda e: e.scalar_tensor_tensor(out=tA[:], in0=tB[:], scalar=sc[:, 2:3], in1=tA[:], op0=ALU.mult, op1=ALU.add),
                                  r=["tA", "tB", "sc"], w=["tA"])
                            tAf = tA[:].rearrange("p a b -> p (a b)")
                            tBf = tB[:].rearrange("p a b -> p (a b)")
                            s.act(lambda e: e.activation(out=tBf, in_=tAf, func=AF.Square), r=["tA"], w=["tB"])
                            s.pe(lambda e: e.matmul(c.psum[7][:], c.ones[:], tBf, start=True, stop=True), r=["tB", "ones"], w=["ps7"])
                            s.act(lambda e: e.activation(out=tBf, in_=c.psum[7][:], func=AF.Sqrt, scale=1.0 / 128, bias=c.epsb[:]),
                                  r=["ps7", "epsb"], w=["tB"])
                            s.dve(lambda e: e.reciprocal(out=tBf, in_=tBf), r=["tB"], w=["tB"])
                            s.dve(lambda e: e.scalar_tensor_tensor(out=OT[:, h, tg * TG:(tg + 1) * TG], in0=tAf, scalar=sc[:, 3:4], in1=tBf,
                                                                   op0=ALU.mult, op1=ALU.mult),
                                  r=["tA", "tB", "sc"], w=["OT"])
                        emit_S(0)
                        for i in range(len(items)):
                            emit_rest(i)
                for h in range(4):
                    a_head(h)
                s.flush()

            if EVEN_DBG == "A":
                s.dve(lambda e: e.memset(OT[:, 4:8, :], 0.0), w=["OT"])
                s.flush()
            for j in (range(4) if EVEN_DBG != "A" else []):
                Wt, wtok, _ = load_w(1536 + j * 128)
                rope_proj(Wt, wtok, lambda tg, j=j: BqT(j)[:, tg * TG:(tg + 1) * TG], "bQ", 0, 0)
                Wt, wtok, _ = load_w(2048 + j * 128)
                rope_proj(Wt, wtok, lambda tg, j=j: BkT(j)[:, tg * TG:(tg + 1) * TG], "bK", 0, 0)
            for j in (range(4) if EVEN_DBG != "A" else []):
                Wt, wtok, _ = load_w(2560 + j * 128)
                for t4 in range(4):
                    bank = c.psum[4 + t4 % 2]
                    bk = f"ps{4 + t4 % 2}"

                    def mmv(e, bank=bank, t4=t4, Wt=Wt):
                        last = None
                        for jj in range(4):
                            tt = t4 * 4 + jj
                            for dc in range(NDC):
                                last = e.matmul(bank[:, jj * 128:(jj + 1) * 128], hT[:, dc, tt * 128:(tt + 1) * 128], Wt[:, dc, :],
                                                start=(dc == 0), stop=(dc == NDC - 1))
                        return last
                    s.pe(mmv, r=[wtok, f"h:{t4}"], w=[bk])
                    for jj in range(4):
                        tt = t4 * 4 + jj
                        s.dve(lambda e, bank=bank, tt=tt, jj=jj, j=j: e.tensor_copy(out=Bv(tt)[:, j * 128:(j + 1) * 128],
                                                                                   in_=bank[:, jj * 128:(jj + 1) * 128]),
                              r=[bk], w=["bV"])
            for j in (range(2) if EVEN_DBG != "A" else []):
                Wt, wtok, _ = load_w(3072 + j * 128)
                rope_proj(Wt, wtok, lambda tg, j=j: qiT[:, j, tg * TG:(tg + 1) * TG], "qi", 2, 1)
            slot = wcnt[0] % 3
            wcnt[0] += 1
            for r4 in (range(4) if EVEN_DBG != "A" else []):
                s.dma("pool", W[slot][:, :, r4 * 32:(r4 + 1) * 32], w_in[:, :, 3328:3360], f"ewk{r4}",
                      r=([] if r4 == 0 else [f"ew{slot}"]), w=([f"ew{slot}", "ewk0"] if r4 == 0 else [f"ewk{r4}"]))
            if EVEN_DBG != "A":
                rope_proj(W[slot], [f"ew{slot}"] + [f"ewk{r4}" for r4 in range(4)], lambda tg: kiT4[:, tg * TG:(tg + 1) * TG], "ki", 2, 1)
            Wt, wtok, _ = load_w(3360, ncol=8)

            def mmw(e):
                last = None
                for tt in range(16):
                    for dc in range(NDC):
                        last = e.matmul(c.psum[5][:, tt * 8:(tt + 1) * 8], hT[:, dc, tt * 128:(tt + 1) * 128], Wt[:, dc, 0:8],
                                        start=(dc == 0), stop=(dc == NDC - 1))
                return last
            s.pe(mmw, r=[wtok] + [f"h:{tg}" for tg in range(4)], w=["ps5"])
            s.act(lambda e: e.activation(out=wq[:], in_=c.psum[5][:, 0:128], func=AF.Copy, scale=float(8 ** -0.5 * 32 ** -0.5)),
                  r=["ps5"], w=["wq"])
            s.flush()

        with ExitStack() as eb:
            def sbb(name, shape, dt):
                return eb.enter_context(_sbt(nc, name, shape, dt))
            NCH = 3
            nm = [sbb(f"b_nm{j}", [128, S], BF16) for j in range(2 * NCH)]
            tb = [sbb(f"b_tb{j}", [128, TG], F32) for j in range(2)]
            m8s = [sbb(f"b_m8{j}", [128, 8], F32) for j in range(NCH)]
            accs = [acc] + [sbb(f"b_acc{j}", [128, S], F32) for j in range(1, NCH)]
            works = [work] + [sbb(f"b_work{j}", [128, S], F32) for j in range(1, NCH)]
            PTb = [sbb(f"b_pt{j}", [128, S], BF16) for j in range(2)]
            recb = [sbb(f"b_rec{j}", [128, 128], F32) for j in range(3)]
            scnt = [0]
            gcnt = [0]
            ocnt = [0]

            def X_thunks(t):
                n = 128 * (t + 1)
                nmt = nm[t % (2 * NCH)]
                ntok = f"nm{t % (2 * NCH)}"
                ch = t % NCH
                acc, work, m8 = accs[ch], works[ch], m8s[ch]
                th = []
                if t < 2 or EVEN_DBG == "BY":
                    def f0():
                        s.dve(lambda e: e.memset(nmt[:, 0:n], 0.0), w=[ntok])
                        s.dve(lambda e: e.memset(nmt[0:64, t * 128 + 64:t * 128 + 128], -30000.0), w=[ntok])
                    th.append(f0)
                    return th
                acct = [f"acc{ch}_{kb}" for kb in range(4)]
                wtok_, mtok_ = f"work{ch}", f"m8{ch}"
                for h in range(8):
                    for kb in range((n + 511) // 512):
                        def fsc(h=h, kb=kb):
                            p0 = (h % 4) * 32
                            ncol = min(512, n - kb * 512)
                            bi = scnt[0] % 2
                            scnt[0] += 1
                            s.pe(lambda e: e.matmul(
                                c.psum[bi][:, 0:ncol], qiT[p0:p0 + 32, h // 4, t * 128:(t + 1) * 128],
                                kiT4[p0:p0 + 32, kb * 512:kb * 512 + ncol], start=True, stop=True, tile_position=(p0, 0)),
                                r=["qi", "ki"], w=[f"ps{bi}"])
                            s.act(lambda e: e.activation(out=tb[bi][:, 0:ncol], in_=c.psum[bi][:, 0:ncol], func=AF.Relu),
                                  r=[f"ps{bi}"], w=[f"tb{bi}"])
                            if h == 0:
                                s.dve(lambda e: e.tensor_scalar_mul(acc[:, kb * 512:kb * 512 + ncol], tb[bi][:, 0:ncol],
                                                                    wq[:, t * 8 + h:t * 8 + h + 1]),
                                      r=[f"tb{bi}", "wq"], w=[acct[kb]])
                            else:
                                s.dve(lambda e: e.scalar_tensor_tensor(
                                    out=acc[:, kb * 512:kb * 512 + ncol], in0=tb[bi][:, 0:ncol],
                                    scalar=wq[:, t * 8 + h:t * 8 + h + 1], in1=acc[:, kb * 512:kb * 512 + ncol],
                                    op0=ALU.mult, op1=ALU.add),
                                    r=[f"tb{bi}", "wq", acct[kb]], w=[acct[kb]])
                        th.append(fsc)
                th.append(lambda: s.dve(lambda e: e.memset(acc[0:64, t * 128 + 64:t * 128 + 128], NEG), r=acct, w=acct))
                for rnd in range(32):
                    def frnd(rnd=rnd):
                        src = acc if rnd == 0 else work
                        stok = acct if rnd == 0 else [wtok_]
                        s.dve(lambda e: e.max(out=m8[:], in_=src[:, 0:n]), r=stok, w=[mtok_])
                        if rnd < 31:
                            s.dve(lambda e: e.match_replace(out=work[:, 0:n], in_to_replace=m8[:], in_values=src[:, 0:n],
                                                            imm_value=NEG),
                                  r=stok + [mtok_], w=[wtok_])
                    th.append(frnd)
                th.append(lambda: s.dve(lambda e: e.tensor_scalar(out=nmt[:, 0:n], in0=acc[:, 0:n], scalar1=m8[:, 7:8], scalar2=-30000.0,
                                                                  op0=ALU.is_lt, op1=ALU.mult),
                                        r=acct + [mtok_], w=[ntok]))
                return th

            def Y_pair(t, hpair):
                nmt = nm[t % (2 * NCH)]
                ntok = f"nm{t % (2 * NCH)}"
                ob = 5 + ocnt[0] % 3
                rj = ocnt[0] % 3
                ocnt[0] += 1
                for h in (2 * hpair, 2 * hpair + 1):
                    r0 = (h % 2) * 64
                    pj = h % 2
                    for g0 in range(0, t + 1, 4):
                        kts = list(range(g0, min(g0 + 4, t + 1)))
                        bi = 2 + gcnt[0] % 3
                        gcnt[0] += 1

                        def fn(e, kts=kts, bi=bi, h=h, r0=r0):
                            last = None
                            for j, kt in enumerate(kts):
                                e.matmul(c.psum[bi][:, j * 128:(j + 1) * 128], BkT(h // 2)[r0:r0 + 64, kt * 128:(kt + 1) * 128],
                                         BqT(h // 2)[r0:r0 + 64, t * 128:(t + 1) * 128], start=True, stop=False)
                                last = e.matmul(c.psum[bi][:, j * 128:(j + 1) * 128], nmt[:, kt * 128:(kt + 1) * 128], c.identb[:],
                                                start=False, stop=True)
                            return last
                        s.pe(fn, r=["bQ", "bK", ntok, "identb"], w=[f"ps{bi}"])
                        nn = len(kts) * 128
                        s.act(lambda e, kts=kts, bi=bi, nn=nn, pj=pj: e.activation(out=PTb[pj][:, kts[0] * 128:kts[0] * 128 + nn],
                                                                                  in_=c.psum[bi][:, 0:nn], func=AF.Exp, scale=0.125),
                              r=[f"ps{bi}"], w=[f"bpt{pj}"])

                    def fnpv(e, h=h, r0=r0, pj=pj):
                        last = None
                        for kt in range(t + 1):
                            e.matmul(c.psum[ob][r0:r0 + 64, 0:128], Bv(kt)[:, h * 64:(h + 1) * 64], PTb[pj][:, kt * 128:(kt + 1) * 128],
                                     start=(kt == 0), stop=(kt == t))
                        for kt in range(t + 1):
                            last = e.matmul(c.psum[ob][r0:r0 + 64, 128:256], c.onesb[:, 0:64], PTb[pj][:, kt * 128:(kt + 1) * 128],
                                            start=(kt == 0), stop=(kt == t))
                        return last
                    s.pe(fnpv, r=[f"bpt{pj}", "bV", "onesb"], w=[f"ps{ob}"])
                s.dve(lambda e: e.reciprocal(out=recb[rj][:], in_=c.psum[ob][:, 128:256]), r=[f"ps{ob}"], w=[f"brec{rj}"])
                s.dve(lambda e: e.tensor_tensor(out=OT[:, 4 + hpair, t * 128:(t + 1) * 128], in0=c.psum[ob][:, 0:128],
                                                in1=recb[rj][:], op=ALU.mult),
                      r=[f"ps{ob}", f"brec{rj}"], w=["OT"])

            if EVEN_DBG in ("A", "AB"):
                s.dve(lambda e: e.memset(OT[:, 4:8, :], 0.0), w=["OT"])
            else:
                from itertools import zip_longest

                def X_group(k):
                    lists = [X_thunks(t) for t in range(k * NCH, min(16, (k + 1) * NCH))]
                    return [f for tup in zip_longest(*lists) for f in tup if f is not None]
                ngroups = (16 + NCH - 1) // NCH
                for f in X_group(0):
                    f()
                for k in range(ngroups):
                    nxt = X_group(k + 1) if k + 1 < ngroups else []
                    ylist = [(t, hpair) for t in range(k * NCH, min(16, (k + 1) * NCH)) for hpair in range(4)]
                    q = (len(nxt) + len(ylist) - 1) // len(ylist)
                    for idx, (t, hpair) in enumerate(ylist):
                        if EVEN_DBG != "BX":
                            Y_pair(t, hpair)
                        for f in nxt[idx * q:(idx + 1) * q]:
                            f()
            s.flush()
        s.dma("sp", c.xT[:].rearrange("p a b -> p (a b)"), c.xspill[:, :], "xsp2", r=["xspill"], w=[f"x:{tg}" for tg in range(4)])
        s.flush()
        out_proj(c, OT, c.e_w_out[0], gi3)


def host_consts(inputs):
    g = np.asarray(inputs["norm_g"], dtype=np.float32)
    g_lay = np.ascontiguousarray(g.reshape(12, 8, 128).transpose(2, 0, 1).reshape(128, 96))
    out = {"g_lay": g_lay, "ident": np.eye(128, dtype=np.float32)}
    rb = np.asarray(inputs["odd_rel_bias"], dtype=np.float32)[0]
    j = np.arange(128)[:, None]
    q = np.arange(640)[None, :]
    idx = np.clip(q - j, -63, 256) + 63
    out["odd_biasT"] = np.ascontiguousarray(rb[:, idx])
    cq, ck = q // 64, j // 64
    out["odd_mask"] = ((ck <= cq) & (cq <= ck + 8)).astype(np.float32)
    t = np.arange(S, dtype=np.float32)[None, :]
    tabs = np.zeros((4, 128, S), np.float32)
    for ti, dim in ((0, 64), (2, 32)):
        inv = (np.float32(10000.0) ** (-np.arange(0, dim, 2, dtype=np.float32) / np.float32(dim))).astype(np.float32)
        fi = (np.arange(128) % dim) % (dim // 2)
        ang = (t * inv[fi][:, None]).astype(np.float32)
        sign = np.where((np.arange(128) % dim) < dim // 2, 1.0, 1.0).astype(np.float32)
        tabs[ti] = np.cos(ang)
        tabs[ti + 1] = np.sin(ang) * sign[:, None]
    out["rope_tabs"] = tabs
    rm = np.zeros((2, 128, 128), np.float32)
    for ri, dim in ((0, 64), (1, 32)):
        for dst in range(128):
            jj = dst % dim
            if jj < dim // 2:
                rm[ri, dst + dim // 2, dst] = -1.0
            else:
                rm[ri, dst - dim // 2, dst] = 1.0
    out["rot_mats"] = rm
    lp = np.asarray(inputs["even_lambda"], dtype=np.float32)[0].reshape(1, 256)
    out["lam_lay"] = np.ascontiguousarray(np.broadcast_to(lp, (128, 256)))
    out["subln_lay"] = np.ascontiguousarray(np.asarray(inputs["even_subln"], dtype=np.float32)[0].reshape(128, 1))
    return out


N_LAUNCH = 1


def kernel(**inputs):
    x = np.ascontiguousarray(np.asarray(inputs["x"], dtype=np.float32))
    consts = host_consts(inputs)
    shared = dict(consts)
    for k in ("ffn_wg", "ffn_wu", "ffn_wd", "even_w_in", "even_w_out", "odd_w_in", "odd_w_out"):
        shared[k] = np.ascontiguousarray(np.asarray(inputs[k], dtype=np.float32))
    nseq = SEQ_PER_CORE // N_LAUNCH
    nc = build_program(nseq, stages=ALL_STAGES)
    out = np.empty_like(x)
    for li in range(N_LAUNCH):
        in_maps = []
        for ci in range(N_CORES):
            m = dict(shared)
            b0 = ci * SEQ_PER_CORE + li * nseq
            m["x"] = x[b0:b0 + nseq]
            in_maps.append(m)
        res = run_bass_kernel_spmd(nc, in_maps, core_ids=list(range(N_CORES)))
        for ci in range(N_CORES):
            b0 = ci * SEQ_PER_CORE + li * nseq
            out[b0:b0 + nseq] = res.results[ci]["y"]
    return out
```
